# Optimizing a Trainium2 kernel written in Bass

```python
import math
import jax, jax.numpy as jnp
from jax import lax
import numpy as np

D_MODEL = 2048
BATCH = 8
SEQ = 2048
DEPTH = 1

MIX_WIDTH = D_MODEL
POOL_WIDTH = MIX_WIDTH // 2
POOL_WINDOWS = (2, 4, 8, 16)
POOL_GROUPS = len(POOL_WINDOWS)
POOL_GROUP_DIM = POOL_WIDTH // POOL_GROUPS
NSA_HEADS = 16
NSA_HEAD_DIM = (MIX_WIDTH - POOL_WIDTH) // NSA_HEADS
NSA_KV_GROUPS = 4
NSA_Q_PER_KV = NSA_HEADS // NSA_KV_GROUPS
KV_WIDTH = NSA_KV_GROUPS * NSA_HEAD_DIM
N_BRANCH = 3
CMP_BLOCK = 32
CMP_STRIDE = 16
CMP_HIDDEN = 256
SEL_BLOCK = 64
N_SEL = 8
WINDOW = 512
Q_BLOCK = 128
FORCE_SCORE = 1e4
IN_SPLITS = (POOL_WIDTH, NSA_HEADS * NSA_HEAD_DIM) + (KV_WIDTH,) * 6 + (NSA_HEADS * N_BRANCH,)
IN_WIDTH = sum(IN_SPLITS)
REL_BUCKETS = 32
REL_MAX_DIST = 128
PEER_HEADS = 8
PEER_N_KEYS = 128
PEER_N_EXPERTS = PEER_N_KEYS * PEER_N_KEYS
PEER_QUERY_DIM = 256
PEER_TOPK = 16
PEER_CHUNK = 128
PLE_DIM = 256
RMS_EPS = 1e-6
NEG_INF = -1e30

kernel_name = 'hybrid_pool_nsa_peer_block'


def rmsnorm(x, w):
    xf = x.astype(jnp.float32)
    y = xf * lax.rsqrt(jnp.mean(xf * xf, axis=-1, keepdims=True) + RMS_EPS)
    return (y * w.astype(jnp.float32)).astype(x.dtype)


def masked_softmax(logits, mask):
    logits = jnp.where(mask, logits.astype(jnp.float32), NEG_INF)
    m = jnp.max(logits, axis=-1, keepdims=True)
    e = jnp.exp(logits - m) * mask
    return e / jnp.maximum(jnp.sum(e, axis=-1, keepdims=True), 1e-30)


def t5_bucket(rel):
    rel = jnp.maximum(rel, 0)
    exact = REL_BUCKETS // 2
    logr = jnp.log(jnp.maximum(rel, 1).astype(jnp.float32) / exact)
    large = exact + (logr / math.log(REL_MAX_DIST / exact) * (REL_BUCKETS - exact)).astype(jnp.int32)
    return jnp.where(rel < exact, rel, jnp.minimum(large, REL_BUCKETS - 1))


def pool_mixer(u, pool_w, pool_scale):
    B, S, _ = u.shape
    ug = u.reshape(B, S, POOL_GROUPS, POOL_GROUP_DIM).astype(jnp.float32)
    csum = jnp.concatenate([jnp.zeros_like(ug[:, :1]), jnp.cumsum(ug, axis=1)], axis=1)
    t = jnp.arange(S)
    pooled = []
    for g, w in enumerate(POOL_WINDOWS):
        lo = jnp.maximum(t + 1 - w, 0)
        cnt = (t + 1 - lo).astype(jnp.float32)
        cg = csum[:, :, g]
        pooled.append((cg[:, 1:] - cg[:, lo]) / cnt[None, :, None])
    mixed = (jnp.stack(pooled, axis=2) - ug).astype(u.dtype)
    y = jnp.einsum('bsgc,gcd->bsgd', mixed, pool_w).reshape(B, S, POOL_WIDTH)
    return y * pool_scale


def compress(kv, pos, w1, b1, w2):
    B, S, G, dk = kv.shape
    r = CMP_BLOCK // CMP_STRIDE
    nc = S // CMP_STRIDE - r + 1
    seg = kv.reshape(B, S // CMP_STRIDE, CMP_STRIDE, G, dk)
    blocks = jnp.concatenate([seg[:, i:i + nc] for i in range(r)], axis=2)
    blocks = blocks + pos[None, None, :, None, :]
    flat = blocks.transpose(0, 1, 3, 2, 4).reshape(B, nc, G, CMP_BLOCK * dk)
    h = jax.nn.gelu(flat @ w1 + b1, approximate=False)
    return h @ w2


def nsa_attention(q, k_cmp, v_cmp, k_slc, v_slc, k_win, v_win, gates, rel_bias):
    B, S, H, dk = q.shape
    G, R = NSA_KV_GROUPS, NSA_Q_PER_KV
    f32 = jnp.float32
    nc = k_cmp.shape[1]
    nsb = S // SEL_BLOCK
    n_sel = min(N_SEL, nsb)
    q = q.reshape(B, S, G, R, dk)
    gates = gates.reshape(B, S, G, R, N_BRANCH)
    bias_t = rel_bias.T.reshape(G, R, REL_BUCKETS).astype(f32)
    cmp_end = jnp.arange(nc) * CMP_STRIDE + CMP_BLOCK - 1
    c_start = np.arange(nc)[:, None] * CMP_STRIDE
    s_start = np.arange(nsb)[None, :] * SEL_BLOCK
    overlap = np.clip(np.minimum(c_start + CMP_BLOCK, s_start + SEL_BLOCK) - np.maximum(c_start, s_start), 0, None) / CMP_STRIDE
    overlap = jnp.asarray(overlap, f32)
    ks_blk = k_slc.reshape(B, nsb, SEL_BLOCK, G, dk).transpose(0, 3, 1, 2, 4)
    vs_blk = v_slc.reshape(B, nsb, SEL_BLOCK, G, dk).transpose(0, 3, 1, 2, 4)
    kw_pad = jnp.pad(k_win, ((0, 0), (WINDOW, 0), (0, 0), (0, 0)))
    vw_pad = jnp.pad(v_win, ((0, 0), (WINDOW, 0), (0, 0), (0, 0)))
    kc = k_cmp.astype(f32)
    vc = v_cmp.astype(f32)
    b_idx = jnp.arange(B)[:, None, None, None]
    g_idx = jnp.arange(G)[None, :, None, None]
    gi5 = jnp.arange(G)[None, :, None, None, None]
    ri5 = jnp.arange(R)[None, None, :, None, None]
    blk = jnp.arange(nsb)
    scale = dk ** -0.5

    def block_fn(qi):
        q0 = qi * Q_BLOCK
        t = q0 + jnp.arange(Q_BLOCK)
        qf = lax.dynamic_slice_in_dim(q, q0, Q_BLOCK, axis=1).astype(f32) * scale
        gb = lax.dynamic_slice_in_dim(gates, q0, Q_BLOCK, axis=1).astype(f32)
        rel_c = t[:, None] - cmp_end[None, :]
        logit_c = jnp.einsum('bqgrd,bcgd->bgrqc', qf, kc) + bias_t[:, :, t5_bucket(rel_c)]
        p_c = masked_softmax(logit_c, rel_c >= 0)
        o_c = jnp.einsum('bgrqc,bcgd->bqgrd', p_c, vc)
        imp = jnp.einsum('bgqc,cj->bgqj', p_c.sum(axis=2), overlap)
        cur = (t // SEL_BLOCK)[:, None]
        forced = (blk[None, :] == 0) | (blk[None, :] == cur) | (blk[None, :] == cur - 1)
        imp = jnp.where(forced, FORCE_SCORE, jnp.where(blk[None, :] <= cur, imp, NEG_INF))
        _, sel = lax.top_k(imp, n_sel)
        n_k = n_sel * SEL_BLOCK
        ks = ks_blk[b_idx, g_idx, sel].reshape(B, G, Q_BLOCK, n_k, dk).astype(f32)
        vs = vs_blk[b_idx, g_idx, sel].reshape(B, G, Q_BLOCK, n_k, dk).astype(f32)
        pos_s = (sel[..., None] * SEL_BLOCK + jnp.arange(SEL_BLOCK)).reshape(B, G, Q_BLOCK, n_k)
        rel_s = t[None, None, :, None] - pos_s
        bias_s = bias_t[gi5, ri5, t5_bucket(rel_s)[:, :, None]]
        logit_s = jnp.einsum('bqgrd,bgqkd->bgrqk', qf, ks) + bias_s
        p_s = masked_softmax(logit_s, (rel_s >= 0)[:, :, None])
        o_s = jnp.einsum('bgrqk,bgqkd->bqgrd', p_s, vs)
        kw = lax.dynamic_slice_in_dim(kw_pad, q0, WINDOW + Q_BLOCK, axis=1).astype(f32)
        vw = lax.dynamic_slice_in_dim(vw_pad, q0, WINDOW + Q_BLOCK, axis=1).astype(f32)
        pos_w = q0 - WINDOW + jnp.arange(WINDOW + Q_BLOCK)
        rel_w = t[:, None] - pos_w[None, :]
        mask_w = (pos_w[None, :] >= 0) & (rel_w >= 0) & (rel_w < WINDOW)
        logit_w = jnp.einsum('bqgrd,bkgd->bgrqk', qf, kw) + bias_t[:, :, t5_bucket(rel_w)]
        p_w = masked_softmax(logit_w, mask_w)
        o_w = jnp.einsum('bgrqk,bkgd->bqgrd', p_w, vw)
        o = gb[..., 0:1] * o_c + gb[..., 1:2] * o_s + gb[..., 2:3] * o_w
        return o.reshape(B, Q_BLOCK, H * dk).astype(q.dtype)

    out = lax.map(block_fn, jnp.arange(S // Q_BLOCK))
    return out.transpose(1, 0, 2, 3).reshape(B, S, H * dk)


def peer_ffn(xn, w_q, sub_keys, u_tab, v_tab):
    B, S, D = xn.shape
    T = B * S
    xt = xn.reshape(T, D)
    qp = (xt @ w_q).reshape(T, PEER_HEADS, 2, PEER_QUERY_DIM // 2).astype(jnp.float32)
    scores = jnp.einsum('thpd,hpkd->thpk', qp, sub_keys.astype(jnp.float32))
    s_top, i_top = lax.top_k(scores, PEER_TOPK)
    n_cand = PEER_TOPK * PEER_TOPK
    cand_s = (s_top[:, :, 0, :, None] + s_top[:, :, 1, None, :]).reshape(T, PEER_HEADS, n_cand)
    cand_i = (i_top[:, :, 0, :, None] * PEER_N_KEYS + i_top[:, :, 1, None, :]).reshape(T, PEER_HEADS, n_cand)
    best_s, best_pos = lax.top_k(cand_s, PEER_TOPK)
    idx = jnp.take_along_axis(cand_i, best_pos, axis=-1)
    g = jax.nn.softmax(best_s, axis=-1).astype(xn.dtype)
    n_chunks = T // PEER_CHUNK

    def chunk_fn(args):
        xc, ic, gc = args
        h = jax.nn.gelu(jnp.einsum('td,thkd->thk', xc, u_tab[ic]), approximate=False)
        return jnp.einsum('thk,thkd->td', gc * h, v_tab[ic])

    out = lax.map(chunk_fn, (xt.reshape(n_chunks, PEER_CHUNK, D),
                             idx.reshape(n_chunks, PEER_CHUNK, PEER_HEADS, PEER_TOPK),
                             g.reshape(n_chunks, PEER_CHUNK, PEER_HEADS, PEER_TOPK)))
    return out.reshape(B, S, D)


def setup_inputs(seed: int = 0) -> dict:
    key = jax.random.key(seed)
    ks = jax.random.split(key, 26)
    f32 = jnp.float32

    def nrm(k, shape, scale):
        return jax.random.normal(k, shape, f32) * scale

    def gain(k, shape):
        return 1.0 + 0.02 * jax.random.normal(k, shape, f32)

    L, D, dk = DEPTH, D_MODEL, NSA_HEAD_DIM
    cmp_in = CMP_BLOCK * dk
    return {
        'x': nrm(ks[0], (BATCH, SEQ, D), 1.0),
        'p': nrm(ks[1], (L, BATCH, SEQ, PLE_DIM), 1.0),
        'rel_bias': nrm(ks[2], (REL_BUCKETS, NSA_HEADS), 0.5),
        'mix_norm_w': gain(ks[3], (L, D)),
        'w_in': nrm(ks[4], (L, D, IN_WIDTH), D ** -0.5),
        'q_norm_w': gain(ks[5], (L, dk)),
        'k_norm_w': gain(ks[6], (L, dk)),
        'cmp_k_pos': nrm(ks[7], (L, CMP_BLOCK, dk), 0.1),
        'cmp_k_w1': nrm(ks[8], (L, cmp_in, CMP_HIDDEN), cmp_in ** -0.5),
        'cmp_k_b1': nrm(ks[9], (L, CMP_HIDDEN), 0.01),
        'cmp_k_w2': nrm(ks[10], (L, CMP_HIDDEN, dk), CMP_HIDDEN ** -0.5),
        'cmp_v_pos': nrm(ks[11], (L, CMP_BLOCK, dk), 0.1),
        'cmp_v_w1': nrm(ks[12], (L, cmp_in, CMP_HIDDEN), cmp_in ** -0.5),
        'cmp_v_b1': nrm(ks[13], (L, CMP_HIDDEN), 0.01),
        'cmp_v_w2': nrm(ks[14], (L, CMP_HIDDEN, dk), CMP_HIDDEN ** -0.5),
        'pool_w': nrm(ks[15], (L, POOL_GROUPS, POOL_GROUP_DIM, POOL_GROUP_DIM), POOL_GROUP_DIM ** -0.5),
        'pool_scale': gain(ks[16], (L, POOL_WIDTH)),
        'w_out': nrm(ks[17], (L, MIX_WIDTH, D), MIX_WIDTH ** -0.5),
        'ffn_norm_w': gain(ks[18], (L, D)),
        'peer_w_q': nrm(ks[19], (L, D, PEER_HEADS * PEER_QUERY_DIM), D ** -0.5),
        'peer_sub_keys': nrm(ks[20], (L, PEER_HEADS, 2, PEER_N_KEYS, PEER_QUERY_DIM // 2), (PEER_QUERY_DIM // 2) ** -0.5),
        'peer_u': nrm(ks[21], (L, PEER_N_EXPERTS, D), D ** -0.5),
        'peer_v': nrm(ks[22], (L, PEER_N_EXPERTS, D), PEER_HEADS ** -0.5),
        'ple_norm_w': gain(ks[23], (L, D)),
        'ple_w_gate': nrm(ks[24], (L, D, D), D ** -0.5),
        'ple_w_proj': nrm(ks[25], (L, PLE_DIM, D), PLE_DIM ** -0.5),
    }


def reference(x, p, rel_bias, mix_norm_w, w_in, q_norm_w, k_norm_w,
              cmp_k_pos, cmp_k_w1, cmp_k_b1, cmp_k_w2,
              cmp_v_pos, cmp_v_w1, cmp_v_b1, cmp_v_w2,
              pool_w, pool_scale, w_out, ffn_norm_w,
              peer_w_q, peer_sub_keys, peer_u, peer_v,
              ple_norm_w, ple_w_gate, ple_w_proj):
    B, S, _ = x.shape
    H, G, dk = NSA_HEADS, NSA_KV_GROUPS, NSA_HEAD_DIM
    offsets = [int(o) for o in np.cumsum(IN_SPLITS)[:-1]]
    for i in range(DEPTH):
        hn = rmsnorm(x, mix_norm_w[i])
        u, q, kc, vc, ksl, vsl, kwn, vwn, gl = jnp.split(hn @ w_in[i], offsets, axis=-1)
        q = rmsnorm(q.reshape(B, S, H, dk), q_norm_w[i])
        k_cmp = rmsnorm(compress(kc.reshape(B, S, G, dk), cmp_k_pos[i], cmp_k_w1[i], cmp_k_b1[i], cmp_k_w2[i]), k_norm_w[i])
        v_cmp = compress(vc.reshape(B, S, G, dk), cmp_v_pos[i], cmp_v_w1[i], cmp_v_b1[i], cmp_v_w2[i])
        k_slc = rmsnorm(ksl.reshape(B, S, G, dk), k_norm_w[i])
        v_slc = vsl.reshape(B, S, G, dk)
        k_win = rmsnorm(kwn.reshape(B, S, G, dk), k_norm_w[i])
        v_win = vwn.reshape(B, S, G, dk)
        gates = jax.nn.sigmoid(gl.reshape(B, S, H, N_BRANCH))
        attn = nsa_attention(q, k_cmp, v_cmp, k_slc, v_slc, k_win, v_win, gates, rel_bias)
        pool = pool_mixer(u, pool_w[i], pool_scale[i])
        x = x + jnp.concatenate([pool, attn], axis=-1) @ w_out[i]
        x = x + peer_ffn(rmsnorm(x, ffn_norm_w[i]), peer_w_q[i], peer_sub_keys[i], peer_u[i], peer_v[i])
        gate = jax.nn.sigmoid(rmsnorm(x, ple_norm_w[i]) @ ple_w_gate[i])
        x = x + gate * (p[i] @ ple_w_proj[i])
    return x
```

```python
import math
from contextlib import ExitStack
import numpy as np
import ml_dtypes
import concourse.bass as bass
import concourse.mybir as mybir
from concourse.bass_utils import run_bass_kernel_spmd

F32 = mybir.dt.float32
BF16 = mybir.dt.bfloat16
AF = mybir.ActivationFunctionType
ALU = mybir.AluOpType
AX = mybir.AxisListType

S = 2048
D = 2048
NT = S // 128
EPS = 1e-6
NEG = -30000.0


class Res:
    __slots__ = ("name", "w", "rs")

    def __init__(self, name):
        self.name = name
        self.w = None
        self.rs = {}


class Sched:
    EPOCH = 30000

    def __init__(self, nc, n_dma=24):
        self.nc = nc
        self.names = ["pe", "act", "dve", "pool", "sp"]
        self.ops = {e: [] for e in self.names}
        self.count = {e: 0 for e in self.names}
        self.epoch = {e: 0 for e in self.names}
        self.seen = {e: {} for e in self.names}
        self.n_dma = {"sp": n_dma, "pool": 12, "act": 4}
        self.dma_next = {q: 0 for q in self.n_dma}
        self.dma_uses = {q: [0] * n for q, n in self.n_dma.items()}
        self.keys = set()
        self.res = {}
        self.out_toks = []
        self.strict_war = True

    def R(self, *name):
        r = self.res.get(name)
        if r is None:
            r = Res(name)
            self.res[name] = r
        return r

    def op(self, e, fn, reads=(), writes=(), dma=False):
        waits = {}
        seen = self.seen[e]

        def need(tok, is_reader):
            if tok is None:
                return
            key, val, teng = tok
            if teng == e and (e == "pe" or (is_reader and not self.strict_war)):
                return
            if seen.get(key, 0) >= val:
                return
            if waits.get(key, 0) < val:
                waits[key] = val

        for r in reads:
            need(r.w, False)
        for w in writes:
            need(w.w, False)
            for tok in w.rs.values():
                need(tok, True)
        if dma:
            s = self.dma_next[e]
            self.dma_next[e] = (s + 1) % self.n_dma[e]
            self.dma_uses[e][s] += 1
            key = ("dma", e, s)
            val = 16 * self.dma_uses[e][s]
            if val > 16:
                need((key, val - 16, None), False)
            tok = (key, val, None)
            inc = (key, 16)
        else:
            if self.count[e] >= self.EPOCH:
                self.epoch[e] += 1
                self.count[e] = 0
            self.count[e] += 1
            key = (e, self.epoch[e])
            tok = (key, self.count[e], e)
            inc = (key, 1)
        self.keys.add(key)
        for k, v in waits.items():
            seen[k] = v
        self.ops[e].append((fn, list(waits.items()), inc))
        for r in reads:
            r.rs[tok[0]] = tok
        for w in writes:
            w.w = tok
            w.rs = {}
        return tok

    def barrier(self):
        toks = []
        for e in self.names:
            for ep in range(self.epoch[e] + 1):
                if (e, ep) in self.keys:
                    toks.append(((e, ep), self.count[e] if ep == self.epoch[e] else self.EPOCH))
        for q, n in self.n_dma.items():
            for s in range(n):
                if self.dma_uses[q][s]:
                    toks.append((("dma", q, s), 16 * self.dma_uses[q][s]))
        for e in self.names:
            seen = self.seen[e]
            waits = []
            for k, v in toks:
                if seen.get(k, 0) < v:
                    waits.append((k, v))
                    seen[k] = v
            if waits:
                self.ops[e].append((None, waits, None))

    def emit(self):
        nc = self.nc
        with ExitStack() as st:
            sems = {}
            for i, k in enumerate(sorted(self.keys, key=str)):
                sems[k] = st.enter_context(nc.semaphore("s%d" % i))
            block = st.enter_context(nc.Block())
            ops = self.ops

            def run(eng, lst):
                for fn, waits, inc in lst:
                    for k, v in waits:
                        eng.wait_ge(sems[k], v)
                    if fn is not None:
                        ins = fn(eng)
                        ins.then_inc(sems[inc[0]], inc[1])

            @block.tensor
            def _(eng):
                run(eng, ops["pe"])

            @block.scalar
            def _(eng):
                run(eng, ops["act"])

            @block.vector
            def _(eng):
                run(eng, ops["dve"])

            @block.gpsimd
            def _(eng):
                run(eng, ops["pool"])

            @block.sync
            def _(eng):
                run(eng, ops["sp"])


class K:
    def __init__(self, nc):
        self.nc = nc
        self.s = Sched(nc)
        self.banks = []
        self.bank_i = 0

    def mm(self, out, lhsT, rhs, start, stop, R, W, **kw):
        return self.s.op("pe", lambda e: e.matmul(out, lhsT, rhs, start=start, stop=stop, **kw), R, W)

    def tr(self, out, in_, ident, R, W):
        return self.s.op("pe", lambda e: e.transpose(out, in_, ident), R, W)

    def act(self, out, in_, func, R, W, **kw):
        return self.s.op("act", lambda e: e.activation(out, in_, func, **kw), R, W)

    def tt(self, out, in0, in1, op, R, W, eng="dve"):
        return self.s.op(eng, lambda e: e.tensor_tensor(out, in0, in1, op), R, W)

    def ts(self, out, in0, s1, s2, op0, op1, R, W, eng="dve", **kw):
        if s2 is None:
            return self.s.op(eng, lambda e: e.tensor_scalar(out, in0, s1, None, op0, **kw), R, W)
        return self.s.op(eng, lambda e: e.tensor_scalar(out, in0, s1, s2, op0, op1, **kw), R, W)

    def stt(self, out, in0, sc, in1, op0, op1, R, W):
        return self.s.op("dve", lambda e: e.scalar_tensor_tensor(out, in0, sc, in1, op0, op1), R, W)

    def cp(self, out, in_, R, W, eng="dve"):
        if eng == "act":
            return self.s.op("act", lambda e: e.copy(out, in_), R, W)
        return self.s.op(eng, lambda e: e.tensor_copy(out, in_), R, W)

    def red(self, out, in_, op, R, W, axis=AX.X):
        return self.s.op("dve", lambda e: e.tensor_reduce(out, in_, axis, op), R, W)

    def recip(self, out, in_, R, W):
        return self.s.op("dve", lambda e: e.reciprocal(out, in_), R, W)

    def max8(self, out, in_, R, W):
        return self.s.op("dve", lambda e: e.max(out, in_), R, W)

    def mrep(self, out, rep, vals, imm, R, W):
        return self.s.op("dve", lambda e: e.match_replace(out, rep, vals, imm), R, W)

    def memset(self, ap, val, W, eng="dve"):
        return self.s.op(eng, lambda e: e.memset(ap, val), (), W)

    def dma(self, out, in_, R, W, q="sp"):
        return self.s.op(q, lambda e: e.dma_start(out=out, in_=in_), R, W, dma=True)

    def bank(self):
        b = self.banks[self.bank_i]
        self.bank_i = (self.bank_i + 1) % len(self.banks)
        return b


def _perm_cols():
    u = list(range(0, 1024))
    q0 = 1024
    kc = list(range(2048, 2304))
    vc = list(range(2304, 2560))
    F = u + kc + vc
    qperm = []
    for c in range(8):
        for h in (c, c + 8):
            qperm += list(range(q0 + h * 64, q0 + (h + 1) * 64))

    def kperm(base):
        o = []
        for g in (0, 2, 1, 3):
            o += list(range(base + g * 64, base + (g + 1) * 64))
        return o

    ksl = kperm(2560)
    vsl = list(range(2816, 3072))
    kwn = kperm(3072)
    vwn = list(range(3328, 3584))
    gl = list(range(3584, 3632))
    T = qperm + ksl + kwn + vsl + vwn + gl
    return np.array(F + T, dtype=np.int64)


def t5_bucket_np(rel):
    rel = np.maximum(rel, 0)
    exact = 16
    logr = np.log(np.maximum(rel, 1).astype(np.float32) / np.float32(exact))
    large = exact + (logr / np.float32(math.log(128 / exact)) * (32 - exact)).astype(np.int32)
    return np.where(rel < exact, rel, np.minimum(large, 31))


def build(debug=()):
    nc = bass.Bass("TRN2", target_bir_lowering=False)
    k = K(nc)
    s = k.s
    dbg = set(debug)

    def din(name, shape, dt=F32):
        return nc.dram_tensor(name, list(shape), dt, kind="ExternalInput").ap()

    def dscr(name, shape, dt):
        kind = "ExternalOutput" if name in dbg else "Internal"
        return nc.dram_tensor(name, list(shape), dt, kind=kind).ap()

    x_d = din("x", [S, D])
    p_d = din("p", [S, 256])
    wF_d = din("w_inF", [12, 128, 16, 128])
    wT_d = din("w_inT", [D, 2096])
    mixw_d = din("mix_norm_w", [1, D])
    qw_d = din("q_norm_w", [1, 64])
    kw_d = din("k_norm_w", [1, 64])
    poolw_d = din("pool_w", [4, 256, 256])
    pools_d = din("pool_scale", [128, 8])
    rc_d = din("rc16", [1, 64])
    ident_d = din("ident", [128, 128])
    out_d = nc.dram_tensor("out", [S, D], F32, kind="ExternalOutput").ap()
    w1_d = din("cmp_w1", [2, 2048, 256])
    b1_d = din("cmp_b1", [128, 4])
    w2_d = din("cmp_w2", [2, 256, 64])
    posT_d = din("cmp_posT", [128, 64])
    ovl_d = din("ovl", [128, 32])
    E2_d = din("E2", [128, S])
    biasg_d = din("bias_g", [128, 6, 2048])
    maskc_d = din("mask_c", [128, 6, 2048])
    biasCg_d = din("biasC_g", [128, 2, 2048])
    maskCc_d = din("maskC_c", [128, 2, 2048])
    cA_d = din("cA", [128, 512])
    cB_d = din("cB", [128, 512])
    wout_d = din("w_out", [D, D])
    ffnw_d = din("ffn_norm_w", [1, D])
    wq_d = din("peer_w_q", [D, D])
    skT_d = din("skT", [128, 16, 128])
    UT_d = din("peer_UT", [64, 128, 16, 256])
    V_d = din("peer_v", [16384, D])
    plew_d = din("ple_norm_w", [1, D])
    wg_d = din("ple_w_gate", [D, D])
    wp_d = din("ple_w_proj", [256, D])
    x1_d = dscr("x1_s", [S, D], F32)
    x2_d = dscr("x2_s", [S, D], F32)
    xn2T_d = dscr("xn2T_s", [128, 16, S], BF16)
    ab_d = dscr("ab_s", [S, 2048], BF16)
    thz_d = dscr("thz_s", [S, 16], F32)
    kcT_d = dscr("kcT_s", [128, 2, 128], BF16)
    vcmp_d = dscr("vcmp_s", [128, 4, 65], BF16)

    qT_d = dscr("qT_s", [128, 8, S], BF16)
    kT_d = dscr("kT_s", [128, 4, S], BF16)
    v_d = dscr("v_s", [S, 520], BF16)
    gates_d = dscr("gates_s", [S, 48], F32)
    kvcT_d = dscr("kvcT_s", [128, 4, S], BF16)
    catT_d = dscr("catT_s", [128, 16, S], BF16)

    with ExitStack() as top:
        def sb(name, shape, dt):
            return top.enter_context(nc.sbuf_tensor(name, list(shape), dt))

        for i in range(8):
            t = top.enter_context(nc.psum_tensor("bank%d" % i, [128, 512], F32))
            k.banks.append((t, s.R("bank", i)))

        ident_f = sb("ident_f", [128, 128], F32)
        ident_b = sb("ident_b", [128, 128], BF16)
        r_id = s.R("ident")
        k.dma(ident_f[:], ident_d, (), (r_id,))
        k.cp(ident_b[:], ident_f[:], (r_id,), (r_id,))

        with ExitStack() as ph:
            def sb1(name, shape, dt):
                return ph.enter_context(nc.sbuf_tensor(name, list(shape), dt))

            w_sb = sb1("w_in_sb", [128, 16, 2096], BF16)
            r_w = s.R("w_in")
            w_view = wT_d.rearrange("(k p) n -> p k n", p=128)
            for kk in range(0, 16, 2):
                k.dma(w_sb[:, kk:kk + 2, :], w_view[:, kk:kk + 2, :], (), (r_w,), q="pool")
            wF_sb = [sb1("wF_sb%d" % i, [128, 16, 128], BF16) for i in range(2)]
            r_wF = [s.R("wF", i) for i in range(2)]
            mixw_sb = sb1("mixw_sb", [128, D], F32)
            r_c = s.R("consts1")
            k.dma(mixw_sb[:], mixw_d.to_broadcast([128, D]), (), (r_c,))
            qw_sb = sb1("qw_sb", [128, 64], F32)
            kw_sb = sb1("kw_sb", [128, 64], F32)
            k.dma(qw_sb[:], qw_d.to_broadcast([128, 64]), (), (r_c,))
            k.dma(kw_sb[:], kw_d.to_broadcast([128, 64]), (), (r_c,))
            rc_sb = sb1("rc_sb", [128, 64], F32)
            k.dma(rc_sb[:], rc_d.to_broadcast([128, 64]), (), (r_c,))
            pools_sb = sb1("pools_sb", [128, 8], F32)
            k.dma(pools_sb[:], pools_d, (), (r_c,))
            poolw_sb = sb1("poolw_sb", [128, 4, 2, 256], BF16)
            k.dma(poolw_sb[:], poolw_d.rearrange("g (kk p) d -> p g kk d", p=128), (), (r_c,), q="pool")

            xt = [sb1("xt%d" % i, [128, D], F32) for i in range(2)]
            r_xt = [s.R("xt", i) for i in range(2)]
            xn = [sb1("xn%d" % i, [128, D], BF16) for i in range(2)]
            r_xn = [s.R("xn", i) for i in range(2)]
            stat = [sb1("stat%d" % i, [128, 4], F32) for i in range(2)]
            r_stat = [s.R("stat", i) for i in range(2)]
            hnT = sb1("hnT", [128, 16, 512], BF16)
            r_hnT = [s.R("hnT", j) for j in range(4)]
            uT = sb1("uT", [128, 8, 528], F32)
            r_uT = s.R("uT")
            pa = sb1("pool_a", [128, 2, 528], F32)
            pb = sb1("pool_b", [128, 2, 528], F32)
            r_pa, r_pb = s.R("pa"), s.R("pb")
            ptmp = sb1("pool_tmp", [128, 2, 16], F32)
            r_ptmp = s.R("ptmp")
            mixT = sb1("mixT", [128, 8, 512], BF16)
            r_mixT = [s.R("mixT", g) for g in range(4)]
            yT = sb1("yT", [128, 8, 512], BF16)
            r_yT = s.R("yT")
            kvc_st = sb1("kvc_st", [128, 4, 512], BF16)
            r_kvc = s.R("kvc_st")
            qT_st = sb1("qT_st", [128, 8, 512], BF16)
            r_qT = s.R("qT_st")
            kT_st = sb1("kT_st", [128, 4, 512], BF16)
            r_kT = s.R("kT_st")
            v_st = [sb1("v_st%d" % i, [128, 8, 65], BF16) for i in range(2)]
            r_vst = [s.R("v_st", i) for i in range(2)]
            for i in range(2):
                k.memset(v_st[i][:, :, 64:65], 1.0, (r_vst[i],))
            g_st = [sb1("g_st%d" % i, [128, 48], F32) for i in range(2)]
            r_gst = [s.R("g_st", i) for i in range(2)]
            sq = sb1("sq", [128, 1024], F32)
            r_sq = s.R("sq")
            junk = sq[:].bitcast(BF16)
            r_junk = r_sq
            qn32 = sb1("qn32", [128, 1024], F32)
            r_qn32 = s.R("qn32")
            qnb = sb1("qnb", [128, 1536], BF16)
            r_qnb = s.R("qnb")
            ss24 = sb1("ss24", [128, 24], F32)
            rs24 = sb1("rs24", [128, 24], F32)
            r_ss = s.R("ss24")

            k.memset(uT[:, :, 0:16], 0.0, (r_uT,))

            for st_i in range(4):
                for j in range(4):
                    tt_i = st_i * 4 + j
                    sl = tt_i % 2
                    k.dma(xt[sl][:], x_d[tt_i * 128:(tt_i + 1) * 128, :], (), (r_xt[sl],))
                    k.act(junk, xt[sl][:], AF.Square, (r_xt[sl],), (r_junk, r_stat[sl]),
                          accum_out=stat[sl][:, 0:1])
                    k.act(stat[sl][:, 1:2], stat[sl][:, 0:1], AF.Sqrt, (r_stat[sl],), (r_stat[sl],),
                          scale=1.0 / D, bias=EPS)
                    k.recip(stat[sl][:, 2:3], stat[sl][:, 1:2], (r_stat[sl],), (r_stat[sl],))
                    k.stt(xn[sl][:], xt[sl][:], stat[sl][:, 2:3], mixw_sb[:], ALU.mult, ALU.mult,
                          (r_xt[sl], r_stat[sl], r_c), (r_xn[sl],))
                    for half in range(2):
                        bt, br = k.bank()
                        bv = bt[:].bitcast(BF16)
                        for c in range(8):
                            kk = half * 8 + c
                            k.tr(bv[:, c * 128:(c + 1) * 128], xn[sl][:, kk * 128:(kk + 1) * 128], ident_b[:],
                                 (r_xn[sl], r_id), (br,))
                        dst = hnT[:, half * 8:(half + 1) * 8, j * 128:(j + 1) * 128]
                        src = bv.rearrange("p (c t) -> p c t", c=8)
                        k.cp(dst, src, (br,), (r_hnT[j],), eng=("act" if half == 0 else "dve"))
                for fc in range(12):
                    wi = fc % 2
                    k.dma(wF_sb[wi][:], wF_d[fc], (), (r_wF[wi],), q="pool")
                    bt, br = k.bank()
                    for kk in range(16):
                        k.mm(bt[:], wF_sb[wi][:, kk, :], hnT[:, kk, :], kk == 0, kk == 15,
                             (r_wF[wi],) + tuple(r_hnT), (br,))
                    if fc < 8:
                        k.cp(uT[:, fc, 16:528], bt[:], (br,), (r_uT,), eng="act")
                    else:
                        k.cp(kvc_st[:, fc - 8, :], bt[:], (br,), (r_kvc,), eng="act")
                k.dma(kvcT_d[:, :, st_i * 512:(st_i + 1) * 512], kvc_st[:], (r_kvc,), (s.R("kvcT_d", st_i),))
                for g in range(4):
                    w = 2 << g
                    cur = uT[:, 2 * g:2 * g + 2, :]
                    r_cur = r_uT
                    step = 1
                    bufs = [(pa, r_pa), (pb, r_pb)]
                    bi = 0
                    while step < w:
                        lo = 2 * step - 1
                        nb, r_nb = bufs[bi]
                        bi ^= 1
                        k.tt(nb[:, :, lo:528], cur[:, :, lo:528], cur[:, :, lo - step:528 - step], ALU.add,
                             (r_cur,), (r_nb,))
                        cur, r_cur = nb, r_nb
                        step *= 2
                    k.stt(mixT[:, 2 * g:2 * g + 2, :], cur[:, :, 16:528], 1.0 / w, uT[:, 2 * g:2 * g + 2, 16:528],
                          ALU.mult, ALU.subtract, (r_cur, r_uT), (r_mixT[g],))
                    if st_i == 0:
                        k.tt(ptmp[:], cur[:, :, 16:32],
                             rc_sb[:, g * 16:(g + 1) * 16].unsqueeze(1).to_broadcast([128, 2, 16]), ALU.mult,
                             (r_cur, r_c), (r_ptmp,))
                        k.tt(mixT[:, 2 * g:2 * g + 2, 0:16], ptmp[:], uT[:, 2 * g:2 * g + 2, 16:32], ALU.subtract,
                             (r_ptmp, r_uT), (r_mixT[g],))
                k.cp(uT[:, :, 0:16], uT[:, :, 512:528], (r_uT,) + tuple(r_mixT), (r_uT,), eng="pool")
                for oc in range(8):
                    g = oc // 2
                    hf = oc % 2
                    bt, br = k.bank()
                    for kk in range(2):
                        k.mm(bt[:], poolw_sb[:, g, kk, hf * 128:(hf + 1) * 128], mixT[:, 2 * g + kk, :],
                             kk == 0, kk == 1, (r_c, r_mixT[g]), (br,))
                    k.act(yT[:, oc, :], bt[:], AF.Copy, (br, r_c), (r_yT,), scale=pools_sb[:, oc:oc + 1])
                k.dma(catT_d[:, 0:8, st_i * 512:(st_i + 1) * 512], yT[:], (r_yT,), (s.R("catT_d", st_i),))
                for j in range(4):
                    tt_i = st_i * 4 + j
                    sl = tt_i % 2
                    T0 = 0
                    segs = [(T0, 512), (T0 + 512, 512), (T0 + 1024, 512), (T0 + 1536, 512), (T0 + 2048, 48)]
                    bks = []
                    for (c0, n) in segs:
                        bt, br = k.bank()
                        for kk in range(16):
                            k.mm(bt[:, 0:n], hnT[:, kk, j * 128:(j + 1) * 128], w_sb[:, kk, c0:c0 + n],
                                 kk == 0, kk == 15, (r_w, r_hnT[j]), (br,))
                        bks.append((bt, br))
                    for bi_, (bt, br) in enumerate(bks[0:3]):
                        k.act(sq[:, 0:512], bt[:], AF.Square, (br,), (r_sq,))
                        k.red(ss24[:, bi_ * 8:(bi_ + 1) * 8], sq[:, 0:512].rearrange("p (h d) -> p h d", d=64), ALU.add,
                              (r_sq,), (r_ss,))
                    k.act(rs24[:], ss24[:], AF.Sqrt, (r_ss,), (r_ss,), scale=1.0 / 64, bias=EPS)
                    k.recip(rs24[:], rs24[:], (r_ss,), (r_ss,))
                    for bi_, (bt, br) in enumerate(bks[0:3]):
                        k.tt(qn32[:, 0:512].rearrange("p (h d) -> p h d", d=64),
                             bt[:].rearrange("p (h d) -> p h d", d=64),
                             rs24[:, bi_ * 8:(bi_ + 1) * 8].unsqueeze(2).to_broadcast([128, 8, 64]), ALU.mult,
                             (br, r_ss), (r_qn32,))
                        wsb = qw_sb if bi_ < 2 else kw_sb
                        k.stt(qnb[:, bi_ * 512:(bi_ + 1) * 512].rearrange("p (h d) -> p h d", d=64),
                              qn32[:, 0:512].rearrange("p (h d) -> p h d", d=64),
                              0.125 if bi_ < 2 else 1.0,
                              wsb[:].unsqueeze(1).to_broadcast([128, 8, 64]), ALU.mult, ALU.mult,
                              (r_qn32, r_c), (r_qnb,))
                    bt, br = k.bank()
                    bv = bt[:].bitcast(BF16)
                    for c in range(8):
                        k.tr(bv[:, c * 128:(c + 1) * 128], qnb[:, c * 128:(c + 1) * 128], ident_b[:], (r_qnb, r_id), (br,))
                    k.cp(qT_st[:, :, j * 128:(j + 1) * 128], bv.rearrange("p (c t) -> p c t", c=8), (br,), (r_qT,),
                         eng="act")
                    bt, br = k.bank()
                    bv = bt[:].bitcast(BF16)
                    for c in range(4):
                        k.tr(bv[:, c * 128:(c + 1) * 128], qnb[:, 1024 + c * 128:1024 + (c + 1) * 128], ident_b[:],
                             (r_qnb, r_id), (br,))
                    k.cp(kT_st[:, :, j * 128:(j + 1) * 128], bv[:, 0:512].rearrange("p (c t) -> p c t", c=4), (br,),
                         (r_kT,), eng="act")
                    bt, br = bks[3]
                    k.cp(v_st[sl][:, :, 0:64], bt[:].rearrange("p (a d) -> p a d", d=64), (br,), (r_vst[sl],), eng="act")
                    k.dma(v_d[tt_i * 128:(tt_i + 1) * 128, :], v_st[sl][:].rearrange("p a d -> p (a d)"), (r_vst[sl],),
                          (s.R("v_d", tt_i),))
                    bt, br = bks[4]
                    k.act(g_st[sl][:], bt[:, 0:48], AF.Sigmoid, (br,), (r_gst[sl],))
                    k.dma(gates_d[tt_i * 128:(tt_i + 1) * 128, :], g_st[sl][:], (r_gst[sl],), (s.R("g_d", tt_i),))
                k.dma(qT_d[:, :, st_i * 512:(st_i + 1) * 512], qT_st[:], (r_qT,), (s.R("qT_d", st_i),))
                k.dma(kT_d[:, :, st_i * 512:(st_i + 1) * 512], kT_st[:], (r_kT,), (s.R("kT_d", st_i),))
        s.barrier()

        all_banks = list(k.banks)
        with ExitStack() as ph3:
            def sb3(name, shape, dt):
                return ph3.enter_context(nc.sbuf_tensor(name, list(shape), dt))

            kcmpT = sb3("kcmpT", [128, 2, 128], BF16)
            r_kcT = s.R("kcmpT")
            vcmp = sb3("vcmp", [128, 4, 65], BF16)
            r_vc = s.R("vcmp")
            kw_sb2 = sb3("kw_sb2", [128, 64], F32)
            r_c3 = s.R("consts3")
            k.dma(kw_sb2[:], kw_d.to_broadcast([128, 64]), (), (r_c3,))
            btab = sb3("btab", [128, 6, 2048], F32)
            bC = sb3("bC", [128, 2, 2048], F32)
            mtmp = [sb3("mtmp%d" % i, [128, 2048], F32) for i in range(2)]
            r_mt = [s.R("mtmp", i) for i in range(2)]
            r_bt = s.R("btab")
            k.dma(btab[:], biasg_d, (), (r_bt,))
            k.dma(bC[:], biasCg_d, (), (r_bt,))
            for i in range(8):
                src_m = maskc_d[:, i, :] if i < 6 else maskCc_d[:, i - 6, :]
                dst_b = btab[:, i, :] if i < 6 else bC[:, i - 6, :]
                k.dma(mtmp[i % 2][:], src_m, (), (r_mt[i % 2],))
                k.tt(dst_b, dst_b, mtmp[i % 2][:], ALU.add, (r_mt[i % 2], r_bt), (r_bt,))
            E2 = sb3("E2_sb", [128, S], BF16)
            k.dma(E2[:], E2_d, (), (r_c3,), q="pool")
            ovl = sb3("ovl_sb", [128, 32], F32)
            cA = sb3("cA_sb", [128, 512], F32)
            cB = sb3("cB_sb", [128, 512], F32)
            k.dma(ovl[:], ovl_d, (), (r_c3,))
            k.dma(cA[:], cA_d, (), (r_c3,))
            k.dma(cB[:], cB_d, (), (r_c3,))
            with ExitStack() as ph2:
                def sb2(name, shape, dt):
                    return ph2.enter_context(nc.sbuf_tensor(name, list(shape), dt))

                kvc = sb2("kvc", [128, 4, S], BF16)
                r_kvc2 = s.R("kvc2")
                k.dma(kvc[:], kvcT_d, (), (r_kvc2,))
                posT = sb2("posT", [128, 64], F32)
                b1 = sb2("b1", [128, 4], F32)
                r_c2 = s.R("consts2")
                k.dma(posT[:], posT_d, (), (r_c2,))
                k.dma(b1[:], b1_d, (), (r_c2,))
                w1 = sb2("w1", [128, 2, 32, 256], BF16)
                w2 = sb2("w2", [128, 2, 2, 64], BF16)
                r_w1 = s.R("w1")
                for kv in range(2):
                    for hb in range(2):
                        k.dma(w1[hb * 64:(hb + 1) * 64, kv], w1_d[kv].rearrange("(j d) c -> d j c", d=64), (), (r_w1,),
                              q="pool")
                    k.dma(w2[:, kv], w2_d[kv].rearrange("(h p) d -> p h d", p=128), (), (r_w1,), q="pool")
                kA = sb2("kA", [128, 4, S], BF16)
                kB = sb2("kB", [128, 4, S], BF16)
                r_kAB = s.R("kAB")
                for c in range(4):
                    kv = c // 2
                    for (dst, o) in ((kA, 0), (kB, 16)):
                        k.tt(dst[:, c, :].rearrange("p (i s) -> p i s", s=16),
                             kvc[:, c, :].rearrange("p (i s) -> p i s", s=16),
                             posT[:, kv * 32 + o:kv * 32 + o + 16].unsqueeze(1).to_broadcast([128, 128, 16]), ALU.add,
                             (r_kvc2, r_c2), (r_kAB,))
                hT = [sb2("hT%d" % i, [128, 2, 128], BF16) for i in range(2)]
                r_hT = [s.R("hT", i) for i in range(2)]
                knb = sb2("knb", [128, 2, 128], BF16)
                r_knb = s.R("knb")
                k.memset(knb[:], 0.0, (r_knb,))
                k.memset(vcmp[:], 0.0, (r_vc,))
                k.memset(vcmp[:, :, 64:65], 1.0, (r_vc,))
                st2 = sb2("st2", [128, 4], F32)
                junk64 = sb2("junk64", [128, 64], F32)
                r_st2 = s.R("st2")
                it = 0
                for kv in range(2):
                    for g in range(4):
                        chunk = 2 * kv + g // 2
                        base = 64 * (g % 2)
                        hi = it % 2
                        it += 1
                        for half in range(2):
                            bt, br = k.bank()
                            for j in range(32):
                                srcT = kA if j < 16 else kB
                                view = srcT[base:base + 64, chunk, :].rearrange("p (i s) -> p i s", s=16)
                                rhs = view[:, 0:127, j] if j < 16 else view[:, 1:128, j - 16]
                                k.mm(bt[:, 0:127], w1[base:base + 64, kv, j, half * 128:(half + 1) * 128], rhs,
                                     j == 0, j == 31, (r_w1, r_kAB), (br,))
                            k.act(hT[hi][:, half, 0:127], bt[:, 0:127], AF.Gelu, (br, r_c2), (r_hT[hi],),
                                  bias=b1[:, kv * 2 + half:kv * 2 + half + 1])
                        bt, br = k.bank()
                        for half in range(2):
                            k.mm(bt[0:127, 0:64], hT[hi][:, half, 0:127], w2[:, kv, half, :], half == 0, half == 1,
                                 (r_hT[hi], r_w1), (br,))
                        if kv == 0:
                            k.act(junk64[0:127, :], bt[0:127, 0:64], AF.Square, (br,), (r_st2,),
                                  accum_out=st2[0:127, 0:1])
                            k.act(st2[0:127, 1:2], st2[0:127, 0:1], AF.Sqrt, (r_st2,), (r_st2,), scale=1.0 / 64, bias=EPS)
                            k.recip(st2[0:127, 2:3], st2[0:127, 1:2], (r_st2,), (r_st2,))
                            k.stt(knb[0:127, g % 2, 64 * (g // 2):64 * (g // 2) + 64], bt[0:127, 0:64], st2[0:127, 2:3],
                                  kw_sb2[0:127, :], ALU.mult, ALU.mult, (br, r_st2, r_c3), (r_knb,))
                        else:
                            k.cp(vcmp[0:127, g, 0:64], bt[0:127, 0:64], (br,), (r_vc,), eng="act")
                bt, br = k.bank()
                bv = bt[:].bitcast(BF16)
                for c in range(2):
                    k.tr(bv[:, c * 128:(c + 1) * 128], knb[:, c, :], ident_b[:], (r_knb, r_id), (br,))
                k.cp(kcmpT[:], bv[:, 0:256].rearrange("p (c t) -> p c t", c=2), (br,), (r_kcT,), eng="act")
                if "kcT_s" in dbg:
                    k.dma(kcT_d, kcmpT[:], (r_kcT,), (s.R("kcT_d"),))
                    k.dma(vcmp_d, vcmp[:], (r_vc,), (s.R("vcmp_d"),))
                s.barrier()
            qT = sb3("qT", [128, 8, S], BF16)
            kT = sb3("kT", [128, 4, S], BF16)
            v_all = sb3("v_all", [128, 16, 520], BF16)
            gates = sb3("gates", [128, 16, 48], F32)
            r_in3 = s.R("in3")
            k.dma(qT[:], qT_d, (), (r_in3,))
            k.dma(kT[:], kT_d, (), (r_in3,))
            k.dma(v_all[:], v_d.rearrange("(t p) n -> p t n", p=128), (), (r_in3,))
            k.dma(gates[:], gates_d.rearrange("(t p) n -> p t n", p=128), (), (r_in3,))
            pen_pad = [sb3("pen_pad%d" % i, [128, 128], BF16) for i in range(2)]
            r_pp = [s.R("pen_pad", i) for i in range(2)]
            for i in range(2):
                k.memset(pen_pad[i][:], 0.0, (r_pp[i],))
            pen2 = sb3("pen2", [128, 512], BF16)
            r_pen2 = s.R("pen2")
            k.memset(pen2[:], 0.0, (r_pen2,))
            NB = 3
            sc = [sb3("sc%d" % i, [128, 512], F32) for i in range(NB)]
            r_sc = [s.R("sc", i) for i in range(NB)]
            pT = [sb3("pT%d" % i, [128, 512], BF16) for i in range(NB)]
            r_pT = [s.R("pT", i) for i in range(NB)]
            pc = sb3("pc", [128, 512], F32)
            pn = sb3("pn", [128, 512], F32)
            pcs = sb3("pcs", [128, 128], F32)
            pcsT = sb3("pcsT", [128, 128], F32)
            pcTb = sb3("pcTb", [128, 512], BF16)
            r_pc, r_pn, r_pcs, r_pcsT, r_pcTb = s.R("pc"), s.R("pn"), s.R("pcs"), s.R("pcsT"), s.R("pcTb")
            impb = sb3("impb", [128, 32], F32)
            m8 = sb3("m8", [128, 8], F32)
            zst = sb3("zst", [128, 20], F32)
            r_imp, r_z = s.R("impb"), s.R("zst")
            oacc = sb3("oacc", [128, 256], F32)
            otmp = sb3("otmp", [128, 256], F32)
            r_oacc, r_otmp = s.R("oacc"), s.R("otmp")
            o_tile = [sb3("o_tile%d" % i, [128, 1024], BF16) for i in range(2)]
            r_ot = [s.R("o_tile", i) for i in range(2)]
            aT_st = [sb3("aT_st%d" % i, [128, 8, 128], BF16) for i in range(2)]
            r_aT = [s.R("aT_st", i) for i in range(2)]

            k.banks = all_banks[6:8]
            k.bank_i = 0
            fixed = all_banks[0:6]
            pen2b = [pen2, sb3("pen2_b", [128, 512], BF16)]
            r_pen2b = [r_pen2, s.R("pen2b")]
            k.memset(pen2b[1][:], 0.0, (r_pen2b[1],))
            r_zA, r_zE = s.R("zstA"), s.R("zstE")
            wi_box = [0]

            def make_iter(qi, g, itn):
                q0 = qi * 128
                base = 64 * (g // 2)
                cq = 4 * (g % 2)
                ck = g % 2
                ob = fixed[0:3] if itn % 2 == 0 else fixed[3:6]
                (oc_t, oc_r), (os_t, os_r), (ow_t, ow_r) = ob
                qrhs = qT[base:base + 64, cq:cq + 4, q0:q0 + 128]
                p2 = pen2b[itn % 2]
                r_p2 = r_pen2b[itn % 2]
                pi = g // 2
                osl = qi % 2

                def gen_chain():
                    bt, br = k.bank()
                    for r in range(4):
                        k.mm(bt[:, r * 128:(r + 1) * 128], qT[base:base + 64, cq + r, q0:q0 + 128],
                             kcmpT[base:base + 64, ck, :], True, True, (r_in3, r_kcT), (br,))
                    j0 = 128 - 8 * qi
                    bCv = bC[:].rearrange("p a (h j) -> p (a h) j", j=256)[:, 4 * g:4 * g + 4, j0:j0 + 128]
                    k.tt(pn[:].rearrange("p (r c) -> p r c", c=128), bt[:].rearrange("p (r c) -> p r c", c=128), bCv,
                         ALU.add, (br, r_bt), (r_pn,))
                    yield
                    for r in range(4):
                        k.act(pc[:, r * 128:(r + 1) * 128], pn[:, r * 128:(r + 1) * 128], AF.Exp, (r_pn,), (r_pc, r_zA),
                              accum_out=zst[:, r:r + 1])
                    yield
                    k.ts(zst[:, 4:8], zst[:, 0:4], 1e-30, None, ALU.max, None, (r_zA,), (r_zA,))
                    k.recip(zst[:, 4:8], zst[:, 4:8], (r_zA,), (r_zA,))
                    bt, br = k.bank()
                    for r in range(4):
                        k.tr(bt[:, r * 128:(r + 1) * 128], pc[:, r * 128:(r + 1) * 128], ident_f[:], (r_pc, r_id), (br,))
                    k.cp(pcTb[:], bt[:], (br,), (r_pcTb,), eng="act")
                    yield
                    for r in range(4):
                        k.mm(oc_t[:, r * 65:(r + 1) * 65], pcTb[:, r * 128:(r + 1) * 128], vcmp[:, g, :], True, True,
                             (r_pcTb, r_vc), (oc_r,))
                    k.tt(pn[:].rearrange("p (r c) -> p r c", c=128), pc[:].rearrange("p (r c) -> p r c", c=128),
                         zst[:, 4:8].unsqueeze(2).to_broadcast([128, 4, 128]), ALU.mult, (r_pc, r_zA), (r_pn,))
                    yield
                    k.red(pcs[:], pn[:].rearrange("p (r c) -> p c r", c=128), ALU.add, (r_pn,), (r_pcs,))
                    yield
                    bt, br = k.bank()
                    k.tr(bt[:, 0:128], pcs[:], ident_f[:], (r_pcs, r_id), (br,))
                    k.cp(pcsT[:], bt[:, 0:128], (br,), (r_pcsT,), eng="act")
                    yield
                    bt, br = k.bank()
                    k.mm(bt[:, 0:32], pcsT[:], ovl[:], True, True, (r_pcsT, r_c3), (br,))
                    k.tt(impb[:], bt[:, 0:32], cA[:, qi * 32:(qi + 1) * 32], ALU.mult, (br, r_c3), (r_imp,))
                    yield
                    k.tt(impb[:], impb[:], cB[:, qi * 32:(qi + 1) * 32], ALU.add, (r_imp, r_c3), (r_imp,))
                    yield
                    k.max8(m8[:], impb[:], (r_imp,), (r_imp,))
                    yield
                    k.ts(pen_pad[pi][:, base:base + 32], impb[:], m8[:, 7:8], NEG, ALU.is_lt, ALU.mult,
                         (r_imp,), (r_pp[pi],))
                    yield
                    bt, br = k.bank()
                    bv = bt[:].bitcast(BF16)
                    k.tr(bv[:, 0:128], pen_pad[pi][:], ident_b[:], (r_pp[pi], r_id), (br,))
                    k.cp(p2[base:base + 64, :].rearrange("p (r q) -> p r q", q=128),
                         bv[base:base + 64, 0:128].unsqueeze(1).to_broadcast([64, 4, 128]), (br,), (r_p2,))
                    yield

                first = {"s": True, "w": True}
                last_kc = {"s": qi, "w": qi}

                def emit_pv(job):
                    brn, kc, idx, wsl = job
                    o_t, o_r = (os_t, os_r) if brn == "s" else (ow_t, ow_r)
                    col0 = (g if brn == "s" else 4 + g) * 65
                    for r in range(4):
                        k.mm(o_t[:, r * 65:(r + 1) * 65], pT[wsl][:, r * 128:(r + 1) * 128],
                             v_all[:, kc, col0:col0 + 65],
                             first[brn] and r == 0, kc == last_kc[brn], (r_pT[wsl], r_in3), (o_r,),
                             skip_group_check=True)
                    first[brn] = False

                def gen_jobs():
                    jobs = [("w", qi + off, -off) for off in range(max(-4, -qi), 1)]
                    for kc in range(0, qi + 1):
                        off = kc - qi
                        jobs.append(("s", kc, 0 if off == 0 else (1 if off == -1 else 5)))
                    prev_job = None
                    for (brn, kc, idx) in jobs:
                        bt, br = k.bank()
                        kch = ck if brn == "s" else 2 + ck
                        need_pen = brn == "s" and kc != qi and qi >= 4
                        k.mm(bt[:], kT[base:base + 64, kch, kc * 128:(kc + 1) * 128], qrhs, True, not need_pen,
                             (r_in3,), (br,))
                        if need_pen:
                            k.mm(bt[:], E2[base:base + 64, kc * 128:(kc + 1) * 128], p2[base:base + 64, :], False,
                                 True, (r_c3, r_p2), (br,))
                        if prev_job is not None:
                            emit_pv(prev_job)
                        wsl = wi_box[0] % NB
                        wi_box[0] += 1
                        k.tt(sc[wsl][:], bt[:], btab[:, idx, g * 512:(g + 1) * 512], ALU.add, (br, r_bt),
                             (r_sc[wsl],))
                        k.act(pT[wsl][:], sc[wsl][:], AF.Exp, (r_sc[wsl],), (r_pT[wsl],))
                        prev_job = (brn, kc, idx, wsl)
                        yield
                    if prev_job is not None:
                        emit_pv(prev_job)

                def gen_combine():
                    for bi_, (o_t, o_r) in enumerate(((oc_t, oc_r), (os_t, os_r), (ow_t, ow_r))):
                        ov = o_t[:, 0:260].rearrange("p (r e) -> p r e", e=65)
                        zz = zst[:, 8 + 4 * bi_:12 + 4 * bi_]
                        k.ts(zz, ov[:, :, 64], 1e-30, None, ALU.max, None, (o_r,), (r_zE,))
                        yield
                        k.recip(zz, zz, (r_zE,), (r_zE,))
                        yield
                        gv = gates[:, qi, :].rearrange("p (h b) -> p h b", b=3)[:, 4 * g:4 * g + 4, bi_]
                        k.tt(zz, zz, gv, ALU.mult, (r_zE, r_in3), (r_zE,))
                        yield
                        zb = zz.unsqueeze(2).to_broadcast([128, 4, 64])
                        if bi_ == 0:
                            k.tt(oacc[:].rearrange("p (r d) -> p r d", d=64), ov[:, :, 0:64], zb, ALU.mult,
                                 (o_r, r_zE), (r_oacc,))
                        else:
                            k.tt(otmp[:].rearrange("p (r d) -> p r d", d=64), ov[:, :, 0:64], zb, ALU.mult,
                                 (o_r, r_zE), (r_otmp,))
                            yield
                            k.tt(oacc[:], oacc[:], otmp[:], ALU.add, (r_oacc, r_otmp), (r_oacc,))
                        yield
                    k.cp(o_tile[osl][:, g * 256:(g + 1) * 256], oacc[:], (r_oacc,), (r_ot[osl],))
                    yield
                    if g == 3:
                        bt, br = k.bank()
                        bv = bt[:].bitcast(BF16)
                        for c in range(8):
                            k.tr(bv[:, c * 128:(c + 1) * 128], o_tile[osl][:, c * 128:(c + 1) * 128], ident_b[:],
                                 (r_ot[osl], r_id), (br,))
                        k.cp(aT_st[osl][:], bv.rearrange("p (c t) -> p c t", c=8), (br,), (r_aT[osl],), eng="act")
                        k.dma(catT_d[:, 8:16, q0:q0 + 128], aT_st[osl][:], (r_aT[osl],), (s.R("catT_a", qi),))
                        yield

                return gen_chain, gen_jobs, gen_combine

            all_it = [(qi, g) for qi in range(NT) for g in range(4)]
            objs = [make_iter(qi, g, n) for n, (qi, g) in enumerate(all_it)]
            N_IT = len(objs)
            for _ in objs[0][0]():
                pass
            for n in range(N_IT):
                queue = []
                if n >= 1:
                    queue.append(objs[n - 1][2]())
                if n + 1 < N_IT:
                    queue.append(objs[n + 1][0]())

                def qstep():
                    while queue:
                        try:
                            next(queue[0])
                            return
                        except StopIteration:
                            queue.pop(0)

                for _ in objs[n][1]():
                    qstep()
                while queue:
                    qstep()
            for _ in objs[N_IT - 1][2]():
                pass
            k.banks = all_banks
            k.bank_i = 0
            s.barrier()
        def norm_tile(c_, src_t, r_src, stat_t, r_stat_, junk_ap, r_junk_, wb, r_wb, dst_bf, r_dst):
            k.act(junk_ap, src_t, AF.Square, (r_src,), (r_junk_, r_stat_), accum_out=stat_t[:, 0:1])
            k.act(stat_t[:, 1:2], stat_t[:, 0:1], AF.Sqrt, (r_stat_,), (r_stat_,), scale=1.0 / D, bias=EPS)
            k.recip(stat_t[:, 2:3], stat_t[:, 1:2], (r_stat_,), (r_stat_,))
            k.stt(dst_bf, src_t, stat_t[:, 2:3], wb, ALU.mult, ALU.mult, (r_src, r_stat_, r_wb), (r_dst,))

        def transpose16(src_bf, r_src, dst3, r_dst, col0):
            for half in range(2):
                bt, br = k.bank()
                bv = bt[:].bitcast(BF16)
                for c in range(8):
                    kk = half * 8 + c
                    k.tr(bv[:, c * 128:(c + 1) * 128], src_bf[:, kk * 128:(kk + 1) * 128], ident_b[:], (r_src, r_id), (br,))
                k.cp(dst3[:, half * 8:(half + 1) * 8, col0:col0 + 128], bv.rearrange("p (c t) -> p c t", c=8), (br,),
                     (r_dst,), eng=("act" if half == 0 else "dve"))

        ph45 = ExitStack()
        wq = ph45.enter_context(nc.sbuf_tensor("wq_sb", [128, 16, D], BF16))
        r_wq = s.R("wq")
        with ExitStack() as ph:
            def sb4(name, shape, dt):
                return ph.enter_context(nc.sbuf_tensor(name, list(shape), dt))

            wo = sb4("wo_sb", [128, 16, D], BF16)
            r_wo = s.R("wo")
            wv = wout_d.rearrange("(k p) n -> p k n", p=128)
            for kk in range(0, 16, 4):
                k.dma(wo[:, kk:kk + 4, :], wv[:, kk:kk + 4, :], (), (r_wo,), q="pool")
            wvq = wq_d.rearrange("(k p) n -> p k n", p=128)
            for kk in range(0, 16, 4):
                k.dma(wq[:, kk:kk + 4, :], wvq[:, kk:kk + 4, :], (), (r_wq,), q="pool")
            ffnw = sb4("ffnw_sb", [128, D], F32)
            r_c4 = s.R("consts4")
            k.dma(ffnw[:], ffnw_d.to_broadcast([128, D]), (), (r_c4,))
            cat = [sb4("cat%d" % i, [128, 16, 128], BF16) for i in range(2)]
            r_cat = [s.R("cat", i) for i in range(2)]
            xt4 = [sb4("xt4_%d" % i, [128, D], F32) for i in range(2)]
            r_xt4 = [s.R("xt4", i) for i in range(2)]
            x1t = [sb4("x1t%d" % i, [128, D], F32) for i in range(2)]
            r_x1t = [s.R("x1t", i) for i in range(2)]
            xn2 = [sb4("xn2_%d" % i, [128, D], BF16) for i in range(2)]
            r_xn2 = [s.R("xn2", i) for i in range(2)]
            junk4 = sb4("junk4", [128, D], BF16)
            r_junk4 = s.R("junk4")
            stat4 = [sb4("stat4_%d" % i, [128, 4], F32) for i in range(2)]
            r_stat4 = [s.R("stat4", i) for i in range(2)]
            xT_st = [sb4("xT_st%d" % i, [128, 16, 512], BF16) for i in range(1)]
            r_xT = [s.R("xT_st", i) for i in range(1)]
            def mm4(tt_i):
                sl = tt_i % 2
                k.dma(cat[sl][:], catT_d[:, :, tt_i * 128:(tt_i + 1) * 128], (), (r_cat[sl],))
                k.dma(xt4[sl][:], x_d[tt_i * 128:(tt_i + 1) * 128, :], (), (r_xt4[sl],))
                for nb in range(4):
                    bt, br = k.bank()
                    for kk in range(16):
                        k.mm(bt[:], cat[sl][:, kk, :], wo[:, kk, nb * 512:(nb + 1) * 512], kk == 0, kk == 15,
                             (r_cat[sl], r_wo), (br,))
                    k.tt(x1t[sl][:, nb * 512:(nb + 1) * 512], bt[:], xt4[sl][:, nb * 512:(nb + 1) * 512], ALU.add,
                         (br, r_xt4[sl]), (r_x1t[sl],))
                k.dma(x1_d[tt_i * 128:(tt_i + 1) * 128, :], x1t[sl][:], (r_x1t[sl],), (s.R("x1_d", tt_i),))

            def post4(tt_i):
                sl = tt_i % 2
                st_i, j = tt_i // 4, tt_i % 4
                ssl = 0
                norm_tile(None, x1t[sl][:], r_x1t[sl], stat4[sl], r_stat4[sl], junk4[:], r_junk4, ffnw[:], r_c4,
                          xn2[sl][:], r_xn2[sl])
                transpose16(xn2[sl], r_xn2[sl], xT_st[ssl], r_xT[ssl], j * 128)
                if j == 3:
                    k.dma(xn2T_d[:, :, st_i * 512:(st_i + 1) * 512], xT_st[ssl][:], (r_xT[ssl],), (s.R("xn2T_d", st_i),))

            mm4(0)
            for tt_i in range(NT):
                if tt_i + 1 < NT:
                    mm4(tt_i + 1)
                post4(tt_i)
        s.barrier()

        with ExitStack() as ph:
            def sb5(name, shape, dt):
                return ph.enter_context(nc.sbuf_tensor(name, list(shape), dt))

            skT = sb5("skT_sb", [128, 16, 128], BF16)
            k.dma(skT[:], skT_d, (), (r_wq,), q="pool")
            xst = [sb5("xst%d" % i, [128, 16, 512], BF16) for i in range(2)]
            r_xst = [s.R("xst", i) for i in range(2)]
            qpT = sb5("qpT", [128, 16, 512], BF16)
            r_qpT = s.R("qpT")
            ab = [sb5("ab%d" % i, [128, 16, 128], BF16) for i in range(2)]
            r_ab = [s.R("ab", i) for i in range(2)]
            abm = [sb5("abm%d" % i, [128, 16, 128], BF16) for i in range(2)]
            r_abm = [s.R("abm", i) for i in range(2)]
            m16 = sb5("m16", [128, 16, 16], F32)
            r_m16s = [s.R("m16", i) for i in range(16)]
            tmpk = [sb5("tmpk%d" % i, [128, 128], BF16) for i in range(4)]
            r_tmpks = [s.R("tmpk", i) for i in range(4)]
            cand = [sb5("cand%d" % i, [128, 256], F32) for i in range(4)]
            candk = [sb5("candk%d" % i, [128, 256], F32) for i in range(4)]
            c16 = [sb5("c16_%d" % i, [128, 16], F32) for i in range(4)]
            r_cs = [s.R("cand", i) for i in range(4)]
            thz = [sb5("thz%d" % i, [128, 16], F32) for i in range(2)]
            r_thz = [s.R("thz", i) for i in range(2)]
            zs5 = [sb5("zs5_%d" % i, [128, 4], F32) for i in range(4)]
            thr16 = sb5("thr16", [128, 16], BF16)
            r_thr = s.R("thr16")
            cand4 = [sb5("cand4_%d" % i, [128, 4, 256], F32) for i in range(2)]
            candk4 = [sb5("candk4_%d" % i, [128, 4, 256], F32) for i in range(2)]
            c16g4 = [sb5("c16g4_%d" % i, [128, 4, 16], F32) for i in range(2)]
            zs4 = [sb5("zs4_%d" % i, [128, 8], F32) for i in range(2)]
            a16s4 = [sb5("a16s4_%d" % i, [128, 4, 16], BF16) for i in range(2)]
            r_c4 = [[s.R("c4", i, j) for j in range(4)] for i in range(2)]
            a16s = [sb5("a16s%d" % i, [128, 16], BF16) for i in range(4)]
            r_abms = [[s.R("abm", i, hp) for hp in range(16)] for i in range(2)]
            r_thzs = [[s.R("thz", i, h) for h in range(8)] for i in range(2)]
            for i in range(2):
                k.memset(thz[i][:], 0.0, tuple(r_thzs[i]))
            qpT2 = [qpT, sb5("qpT_b", [128, 16, 512], BF16)]
            r_qpT2 = [r_qpT, s.R("qpT_b")]

            def emit_qp(st_i):
                ssl = st_i % 2
                k.dma(xst[ssl][:], xn2T_d[:, :, st_i * 512:(st_i + 1) * 512], (), (r_xst[ssl],))
                for hp in range(16):
                    bt, br = k.bank()
                    for kk in range(16):
                        k.mm(bt[:], wq[:, kk, hp * 128:(hp + 1) * 128], xst[ssl][:, kk, :], kk == 0, kk == 15,
                             (r_wq, r_xst[ssl]), (br,))
                    k.cp(qpT2[ssl][:, hp, :], bt[:], (br,), (r_qpT2[ssl],), eng="act")

            emit_qp(0)
            for st_i in range(4):
                ssl = st_i % 2
                qpT = qpT2[ssl]
                r_qpT = r_qpT2[ssl]
                for j in range(4):
                    tt_i = st_i * 4 + j
                    sl = tt_i % 2
                    for b4 in range(4):
                        bt, br = k.bank()
                        for i4 in range(4):
                            hp = b4 * 4 + i4
                            k.mm(bt[:, i4 * 128:(i4 + 1) * 128], qpT[:, hp, j * 128:(j + 1) * 128], skT[:, hp, :], True, True,
                                 (r_qpT, r_wq), (br,))
                        k.act(ab[sl][:, b4 * 4:(b4 + 1) * 4, :], bt[:].rearrange("p (a c) -> p a c", c=128), AF.Exp,
                              (br,), (r_ab[sl],))
                    if j == 0 and st_i + 1 < 4:
                        emit_qp(st_i + 1)
                    NI = 4
                    for hp0 in range(0, 16, NI):
                        hps = list(range(hp0, hp0 + NI))
                        for hp in hps:
                            k.max8(m16[:, hp, 0:8], ab[sl][:, hp, :], (r_ab[sl],), (r_m16s[hp],))
                        for hp in hps:
                            k.mrep(tmpk[hp % NI][:], m16[:, hp, 0:8], ab[sl][:, hp, :], 0.0, (r_m16s[hp], r_ab[sl]),
                                   (r_tmpks[hp % NI],))
                        for hp in hps:
                            k.max8(m16[:, hp, 8:16], tmpk[hp % NI][:], (r_tmpks[hp % NI],), (r_m16s[hp],))
                    k.cp(thr16[:], m16[:, :, 15], tuple(r_m16s), (r_thr,))
                    k.tt(abm[sl][:], ab[sl][:], thr16[:].unsqueeze(2).to_broadcast([128, 16, 128]), ALU.is_ge,
                         (r_ab[sl], r_thr), tuple(r_abms[sl]))
                    k.tt(abm[sl][:], abm[sl][:], ab[sl][:], ALU.mult, (r_ab[sl],) + tuple(r_abms[sl]), tuple(r_abms[sl]))
                    m16v = m16[:].rearrange("p (h x) k -> p h x k", x=2)
                    abmv = abm[sl][:].rearrange("p (h x) k -> p h x k", x=2)
                    for gi, h0 in enumerate((0, 4)):
                        hs = list(range(h0, h0 + 4))
                        A = m16v[:, h0:h0 + 4, 0, :]
                        Bm = m16v[:, h0:h0 + 4, 1, :]
                        rA = tuple(r_m16s[2 * h] for h in hs)
                        rB = tuple(r_m16s[2 * h + 1] for h in hs)
                        rc = tuple(r_c4[gi])
                        c4 = cand4[gi]
                        ck4 = candk4[gi]
                        c16g = c16g4[gi]
                        zsg = zs4[gi]

                        def top16():
                            for j in range(4):
                                k.max8(c16g[:, j, 0:8], c4[:, j, :], (r_c4[gi][j],), (r_c4[gi][j],))
                            for j in range(4):
                                k.mrep(ck4[:, j, :], c16g[:, j, 0:8], c4[:, j, :], 0.0, (r_c4[gi][j],), (r_c4[gi][j],))
                            for j in range(4):
                                k.max8(c16g[:, j, 8:16], ck4[:, j, :], (r_c4[gi][j],), (r_c4[gi][j],))

                        k.tt(c4[:].rearrange("p h (a b) -> p h a b", b=16),
                             A.unsqueeze(3).to_broadcast([128, 4, 16, 16]),
                             Bm.unsqueeze(2).to_broadcast([128, 4, 16, 16]), ALU.mult, rA + rB + rc, rc)
                        top16()
                        k.red(zsg[:, 0:4], c16g[:], ALU.add, rc, rc)
                        k.recip(zsg[:, 4:8], zsg[:, 0:4], rc, rc)
                        rabm = tuple(r_abms[sl][2 * h] for h in hs)
                        k.tt(abmv[:, h0:h0 + 4, 0, :], abmv[:, h0:h0 + 4, 0, :],
                             zsg[:, 4:8].unsqueeze(2).to_broadcast([128, 4, 128]), ALU.mult, rc + rabm, rabm)
                        k.tt(a16s4[gi][:], A, zsg[:, 4:8].unsqueeze(2).to_broadcast([128, 4, 16]), ALU.mult,
                             rA + rc, rc)
                        k.tt(c4[:].rearrange("p h (a b) -> p h a b", b=16),
                             a16s4[gi][:].unsqueeze(3).to_broadcast([128, 4, 16, 16]),
                             Bm.unsqueeze(2).to_broadcast([128, 4, 16, 16]), ALU.mult, rB + rc, rc)
                        top16()
                        rth = tuple(r_thzs[sl][h] for h in hs)
                        k.ts(thz[sl][:, h0:h0 + 4], c16g[:, :, 15], 0.99999, None, ALU.mult, None, rc, rth)
                        k.recip(thz[sl][:, 8 + h0:12 + h0], thz[sl][:, h0:h0 + 4], rth, rth)
                    k.dma(ab_d[tt_i * 128:(tt_i + 1) * 128, :], abm[sl][:].rearrange("p a c -> p (a c)"),
                          tuple(r_abms[sl]), (s.R("ab_d", tt_i),))
                    k.dma(thz_d[tt_i * 128:(tt_i + 1) * 128, :], thz[sl][:], tuple(r_thzs[sl]), (s.R("thz_d", tt_i),))
        s.barrier()
        ph45.close()

        with ExitStack() as ph:
            def sb6(name, shape, dt):
                return ph.enter_context(nc.sbuf_tensor(name, list(shape), dt))

            xh = sb6("xh", [128, 16, 1024], BF16)
            acc = sb6("acc", [128, 8, D], F32)
            abh = sb6("abh", [128, 8, 2048], BF16)
            thh = sb6("thh", [128, 8, 16], F32)
            r_xh, r_abh = s.R("xh"), s.R("abh")
            r_acc = [[s.R("acc", i, nb) for nb in range(4)] for i in range(8)]
            ub = [sb6("ub%d" % i, [128, 16, 256], BF16) for i in range(2)]
            vb = [sb6("vb%d" % i, [128, 2, D], BF16) for i in range(2)]
            r_ub = [s.R("ub", i) for i in range(2)]
            r_vb = [s.R("vb", i) for i in range(2)]
            NW = 2
            NT6 = 3
            tmp6 = [sb6("tmp6_%d" % i, [128, 2048], F32) for i in range(NT6)]
            r_tmp6 = [s.R("tmp6", i) for i in range(NT6)]
            msk = [sb6("msk%d" % i, [128, 2048], BF16) for i in range(NW)]
            r_msk = [s.R("msk", i) for i in range(NW)]
            mvb = [sb6("mvb%d" % i, [128, 1024], BF16) for i in range(NW)]
            r_mvb = [s.R("mvb", i) for i in range(NW)]
            ghT = [sb6("ghT%d" % i, [128, 2, 1024], BF16) for i in range(2)]
            r_ghT = [[s.R("ghT", i, q) for q in range(4)] for i in range(2)]
            WhT = [sb6("WhT%d" % i, [128, 256], BF16) for i in range(2)]
            r_WhT = [s.R("WhT", i) for i in range(2)]
            NEB = 64
            def load_half(half):
                t0 = half * 1024
                k.dma(xh[:], xn2T_d[:, :, t0:t0 + 1024], (), (r_xh,))
                k.dma(abh[:], ab_d[t0:t0 + 1024, :].rearrange("(t p) n -> p t n", p=128), (), (r_abh,))
                k.dma(thh[:], thz_d[t0:t0 + 1024, :].rearrange("(t p) n -> p t n", p=128), (), (r_abh,))

            load_half(0)
            for half in range(2):
                t0 = half * 1024
                iters = [(eb, tt_i) for eb in range(NEB) for tt_i in range(8)]

                def load_u(eb):
                    k.dma(ub[eb % 2][:], UT_d[eb], (), (r_ub[eb % 2],), q="pool")

                def load_v(eb):
                    k.dma(vb[eb % 2][:], V_d[eb * 256:(eb + 1) * 256, :].rearrange("(c p) d -> p c d", p=128), (),
                          (r_vb[eb % 2],), q="pool")

                hT_pending = []

                hT_open = {}

                def emit_hT_mm(eb, q, part=None):
                    c, grp = q // 2, q % 2
                    bsl = eb % 2
                    if part in (None, 0):
                        hT_open[(eb, q)] = k.bank()
                    bt, br = hT_open[(eb, q)]
                    kks = range(16) if part is None else (range(8) if part == 0 else range(8, 16))
                    for kk in kks:
                        k.mm(bt[:], ub[bsl][:, kk, c * 128:(c + 1) * 128], xh[:, kk, grp * 512:(grp + 1) * 512],
                             kk == 0, kk == 15, (r_xh, r_ub[bsl]), (br,))
                    if part in (None, 1):
                        del hT_open[(eb, q)]
                        hT_pending.append((eb, q, bt, br))

                def emit_hT_gelu():
                    while hT_pending:
                        eb, q, bt, br = hT_pending.pop(0)
                        c, grp = q // 2, q % 2
                        bsl = eb % 2
                        k.act(ghT[bsl][:, c, grp * 512:(grp + 1) * 512], bt[:], AF.Gelu, (br,), (r_ghT[bsl][q],))

                def emit_outer(it):
                    eb, tt_i = iters[it]
                    w_ = it % NT6
                    av = abh[:, tt_i, :].rearrange("p (h x k) -> p h x k", x=2, k=128)
                    a_ap = av[:, :, 0, 2 * eb:2 * eb + 2]
                    b_ap = av[:, :, 1, :]
                    t4 = tmp6[w_][:].rearrange("p (h a b) -> p h a b", a=2, b=128)
                    k.tt(t4, a_ap.unsqueeze(3).to_broadcast([128, 8, 2, 128]),
                         b_ap.unsqueeze(2).to_broadcast([128, 8, 2, 128]), ALU.mult, (r_abh,), (r_tmp6[w_],), eng="pool")

                def emit_signs(it):
                    eb, tt_i = iters[it]
                    w_ = it % NW
                    t_ = it % NT6
                    for h in range(8):
                        k.act(msk[w_][:, h * 256:(h + 1) * 256], tmp6[t_][:, h * 256:(h + 1) * 256], AF.Sign,
                              (r_tmp6[t_], r_abh), (r_msk[w_],), scale=thh[:, tt_i, 8 + h:9 + h], bias=-1.0)

                def emit_stt(it):
                    w_ = it % NW
                    t_ = it % NT6
                    k.stt(msk[w_][:], msk[w_][:], 0.0, tmp6[t_][:], ALU.max, ALU.mult, (r_tmp6[t_], r_msk[w_]),
                          (r_msk[w_],))
                    k.tt(mvb[w_][:], msk[w_][:, 0:1024], msk[w_][:, 1024:2048], ALU.add, (r_msk[w_],),
                         (r_mvb[w_],))

                def emit_headsum(it):
                    w_ = it % NW
                    btw, brw = k.bank()
                    for c in range(2):
                        for h in range(4):
                            k.mm(btw[:, c * 128:(c + 1) * 128],
                                 mvb[w_][:, h * 256 + c * 128:h * 256 + (c + 1) * 128], ident_b[:], h == 0, h == 3,
                                 (r_mvb[w_], r_id), (brw,))
                    return (btw, brw)

                def emit_WhT(it, bw):
                    eb, tt_i = iters[it]
                    bsl = eb % 2
                    g_ = it % 2
                    btw, brw = bw
                    grp = tt_i // 4
                    k.tt(WhT[g_][:].rearrange("p (c t) -> p c t", c=2), btw[:, 0:256].rearrange("p (c t) -> p c t", c=2),
                         ghT[bsl][:, :, tt_i * 128:(tt_i + 1) * 128], ALU.mult,
                         (brw, r_ghT[bsl][grp], r_ghT[bsl][2 + grp]), (r_WhT[g_],))

                def emit_V(it):
                    eb, tt_i = iters[it]
                    bsl = eb % 2
                    g_ = it % 2
                    vbanks = []
                    for nb in range(4):
                        bt, br = k.bank()
                        for c in range(2):
                            k.mm(bt[:], WhT[g_][:, c * 128:(c + 1) * 128], vb[bsl][:, c, nb * 512:(nb + 1) * 512],
                                 c == 0, c == 1, (r_WhT[g_], r_vb[bsl]), (br,))
                        vbanks.append((bt, br))
                    return vbanks

                def emit_adds(it, vbanks):
                    eb, tt_i = iters[it]
                    for nb in range(4):
                        bt, br = vbanks[nb]
                        dst = acc[:, tt_i, nb * 512:(nb + 1) * 512]
                        ra = r_acc[tt_i][nb]
                        if eb == 0:
                            k.cp(dst, bt[:], (br,), (ra,))
                        else:
                            k.tt(dst, bt[:], dst, ALU.add, (br, ra), (ra,))

                n_it = len(iters)
                load_u(0)
                load_u(1)
                load_v(0)
                emit_outer(0)
                emit_outer(1)
                emit_outer(2)
                emit_signs(0)
                emit_signs(1)
                emit_stt(0)
                for q in range(4):
                    emit_hT_mm(0, q)
                emit_hT_gelu()
                bw_q = {}
                v_q = {}
                for i in range(n_it + 3):
                    if i < n_it:
                        eb, tt_i = iters[i]
                        if tt_i == 3 and eb + 1 < NEB:
                            load_v(eb + 1)
                        if tt_i == 3 and eb + 2 < NEB:
                            load_u(eb + 2)
                        bw_q[i] = emit_headsum(i)
                    if 0 <= i - 1 < n_it:
                        emit_WhT(i - 1, bw_q.pop(i - 1))
                    if 0 <= i - 3 < n_it:
                        emit_adds(i - 3, v_q.pop(i - 3))
                    if 0 <= i - 2 < n_it:
                        v_q[i - 2] = emit_V(i - 2)
                    if i < n_it:
                        eb, tt_i = iters[i]
                        if eb + 1 < NEB:
                            emit_hT_mm(eb + 1, tt_i // 2, tt_i % 2)
                    if i + 3 < n_it:
                        emit_outer(i + 3)
                    if i + 2 < n_it:
                        emit_signs(i + 2)
                    emit_hT_gelu()
                    if i + 1 < n_it:
                        emit_stt(i + 1)
                if half == 0:
                    load_half(1)

                def x1_load(tt_i):
                    gt = half * 8 + tt_i
                    sl = tt_i % NT6
                    k.dma(tmp6[sl][:], x1_d[gt * 128:(gt + 1) * 128, :], (), (r_tmp6[sl],))

                for tt_i in range(min(NT6, 8)):
                    x1_load(tt_i)
                for tt_i in range(8):
                    gt = half * 8 + tt_i
                    sl = tt_i % NT6
                    x1v = tmp6[sl][:]
                    k.tt(x1v, x1v, acc[:, tt_i, :], ALU.add, (r_tmp6[sl],) + tuple(r_acc[tt_i]), (r_tmp6[sl],))
                    k.dma(x2_d[gt * 128:(gt + 1) * 128, :], x1v, (r_tmp6[sl],), (s.R("x2_d", gt),))
                    if tt_i + NT6 < 8:
                        x1_load(tt_i + NT6)
        s.barrier()

        with ExitStack() as ph:
            def sb7(name, shape, dt):
                return ph.enter_context(nc.sbuf_tensor(name, list(shape), dt))

            wg = sb7("wg_sb", [128, 16, D], BF16)
            wp = sb7("wp_sb", [128, 2, D], BF16)
            r_wg = s.R("wg")
            wv = wg_d.rearrange("(k p) n -> p k n", p=128)
            for kk in range(0, 16, 4):
                k.dma(wg[:, kk:kk + 4, :], wv[:, kk:kk + 4, :], (), (r_wg,), q="pool")
            k.dma(wp[:], wp_d.rearrange("(k p) n -> p k n", p=128), (), (r_wg,), q="pool")
            plew = sb7("plew_sb", [128, D], F32)
            r_c7 = s.R("consts7")
            k.dma(plew[:], plew_d.to_broadcast([128, D]), (), (r_c7,))
            x2t = [sb7("x2t%d" % i, [128, D], F32) for i in range(2)]
            r_x2t = [s.R("x2t", i) for i in range(2)]
            pt = [sb7("pt%d" % i, [128, 256], F32) for i in range(2)]
            ptb = [sb7("ptb%d" % i, [128, 256], BF16) for i in range(2)]
            r_pt = [s.R("pt", i) for i in range(2)]
            r_ptb = [s.R("ptb", i) for i in range(2)]
            xn3 = [sb7("xn3_%d" % i, [128, D], BF16) for i in range(2)]
            r_xn3 = [s.R("xn3", i) for i in range(2)]
            junk7 = sb7("junk7", [128, D], BF16)
            r_junk7 = s.R("junk7")
            stat7 = [sb7("stat7_%d" % i, [128, 4], F32) for i in range(2)]
            r_stat7 = [s.R("stat7", i) for i in range(2)]
            xn3T = [sb7("xn3T%d" % i, [128, 16, 128], BF16) for i in range(2)]
            r_xn3T = [s.R("xn3T", i) for i in range(2)]
            ppT = [sb7("ppT%d" % i, [128, 2, 128], BF16) for i in range(2)]
            r_ppT = [s.R("ppT", i) for i in range(2)]
            gsb = [sb7("gsb%d" % i, [128, 512], F32) for i in range(2)]
            r_gsb = [s.R("gsb", i) for i in range(2)]
            ot = [sb7("ot%d" % i, [128, D], F32) for i in range(2)]
            r_ot7 = [s.R("ot7", i) for i in range(2)]
            gi = 0

            def prep7(tt_i):
                sl = tt_i % 2
                k.dma(x2t[sl][:], x2_d[tt_i * 128:(tt_i + 1) * 128, :], (), (r_x2t[sl],))
                k.dma(pt[sl][:], p_d[tt_i * 128:(tt_i + 1) * 128, :], (), (r_pt[sl],))
                norm_tile(None, x2t[sl][:], r_x2t[sl], stat7[sl], r_stat7[sl], junk7[:], r_junk7, plew[:], r_c7,
                          xn3[sl][:], r_xn3[sl])
                transpose16(xn3[sl], r_xn3[sl], xn3T[sl], r_xn3T[sl], 0)
                k.cp(ptb[sl][:], pt[sl][:], (r_pt[sl],), (r_ptb[sl],), eng="pool")
                bt, br = k.bank()
                bv = bt[:].bitcast(BF16)
                for c in range(2):
                    k.tr(bv[:, c * 128:(c + 1) * 128], ptb[sl][:, c * 128:(c + 1) * 128], ident_b[:], (r_ptb[sl], r_id), (br,))
                k.cp(ppT[sl][:], bv[:, 0:256].rearrange("p (c t) -> p c t", c=2), (br,), (r_ppT[sl],), eng="act")

            prep7(0)
            for tt_i in range(NT):
                sl = tt_i % 2
                if tt_i + 1 < NT:
                    prep7(tt_i + 1)
                for nb in range(4):
                    g_ = gi % 2
                    gi += 1
                    btg, brg = k.bank()
                    for kk in range(16):
                        k.mm(btg[:], xn3T[sl][:, kk, :], wg[:, kk, nb * 512:(nb + 1) * 512], kk == 0, kk == 15,
                             (r_xn3T[sl], r_wg), (brg,))
                    btp, brp = k.bank()
                    for c in range(2):
                        k.mm(btp[:], ppT[sl][:, c, :], wp[:, c, nb * 512:(nb + 1) * 512], c == 0, c == 1,
                             (r_ppT[sl], r_wg), (brp,))
                    k.act(gsb[g_][:], btg[:], AF.Sigmoid, (brg,), (r_gsb[g_],))
                    k.tt(gsb[g_][:], gsb[g_][:], btp[:], ALU.mult, (r_gsb[g_], brp), (r_gsb[g_],))
                    k.tt(ot[sl][:, nb * 512:(nb + 1) * 512], gsb[g_][:], x2t[sl][:, nb * 512:(nb + 1) * 512], ALU.add,
                         (r_gsb[g_], r_x2t[sl]), (r_ot7[sl],))
                k.dma(out_d[tt_i * 128:(tt_i + 1) * 128, :], ot[sl][:], (r_ot7[sl],), (s.R("out", tt_i),))
        s.barrier()
        s.emit()
    return nc


def _attn_consts(rel_bias):
    f32 = np.float32
    rb = np.asarray(rel_bias, f32)
    sl = np.arange(128)[:, None]
    tl = np.arange(128)[None, :]
    bias_g = np.zeros((128, 6, 4, 4, 128), f32)
    mask_c = np.zeros((128, 6, 4, 4, 128), f32)
    for i in range(5):
        rel = tl - sl + 128 * i
        bk = t5_bucket_np(rel)
        ok = (rel >= 0) & (rel < 512)
        for g in range(4):
            for r in range(4):
                bias_g[:, i, g, r, :] = rb[bk, 4 * g + r]
                mask_c[:, i, g, r, :] = np.where(ok, 0.0, NEG)
    for g in range(4):
        for r in range(4):
            bias_g[:, 5, g, r, :] = rb[31, 4 * g + r]
    tq = np.arange(128)[:, None]
    jj = np.arange(256)[None, :]
    relc = tq - 16 * (jj - 128) - 31
    bkc = t5_bucket_np(relc)
    biasC = np.zeros((128, 16, 256), f32)
    maskC = np.zeros((128, 16, 256), f32)
    for h in range(16):
        biasC[:, h, :] = rb[bkc, h]
        maskC[:, h, :] = np.where(relc >= 0, 0.0, NEG)
    cA = np.zeros((128, 16, 32), f32)
    cB = np.zeros((128, 16, 32), f32)
    blk = np.arange(32)[None, :]
    for qi in range(16):
        t = 128 * qi + np.arange(128)[:, None]
        cur = t // 64
        forced = (blk == 0) | (blk == cur) | (blk == cur - 1)
        valid = blk <= cur
        cA[:, qi, :] = np.where(valid & ~forced, 1.0, 0.0)
        cB[:, qi, :] = np.where(forced, 1e4, np.where(valid, 0.0, -1e30))
    c_start = np.arange(128)[:, None] * 16
    s_start = np.arange(32)[None, :] * 64
    ovl = np.clip(np.minimum(c_start + 32, s_start + 64) - np.maximum(c_start, s_start), 0, None) / 16.0
    ovl = ovl.astype(f32)
    ovl[127, :] = 0.0
    E2 = np.zeros((128, 2048), f32)
    for b_ in range(32):
        E2[b_, b_ * 64:(b_ + 1) * 64] = 1.0
        E2[64 + b_, b_ * 64:(b_ + 1) * 64] = 1.0
    return {
        "bias_g": bias_g.reshape(128, 6, 2048), "mask_c": mask_c.reshape(128, 6, 2048),
        "biasC_g": biasC.reshape(128, 2, 2048), "maskC_c": maskC.reshape(128, 2, 2048),
        "cA": cA.reshape(128, 512), "cB": cB.reshape(128, 512), "ovl": ovl, "E2": E2,
    }


def host_inputs(inputs):
    perm = _perm_cols()
    f32 = np.float32
    w_in = inputs["w_in"][0][:, perm]
    wF = np.ascontiguousarray(w_in[:, 0:1536].reshape(16, 128, 12, 128).transpose(2, 1, 0, 3))
    wT = np.ascontiguousarray(w_in[:, 1536:])
    pools = np.ascontiguousarray(inputs["pool_scale"][0].reshape(8, 128).T)
    rc = np.zeros((4, 16), f32)
    for g in range(4):
        w = 2 << g
        for t in range(16):
            rc[g, t] = 1.0 / min(t + 1, w)
    common = {
        "w_inF": wF,
        "w_inT": wT,
        "mix_norm_w": np.ascontiguousarray(inputs["mix_norm_w"][0:1]),
        "q_norm_w": np.ascontiguousarray(inputs["q_norm_w"][0:1]),
        "k_norm_w": np.ascontiguousarray(inputs["k_norm_w"][0:1]),
        "pool_w": np.ascontiguousarray(inputs["pool_w"][0]),
        "pool_scale": pools,
        "rc16": rc.reshape(1, 64),
        "ident": np.eye(128, dtype=f32),
    }
    common.update(_attn_consts(inputs["rel_bias"]))
    common["w_out"] = np.ascontiguousarray(inputs["w_out"][0])
    common["ffn_norm_w"] = np.ascontiguousarray(inputs["ffn_norm_w"][0:1])
    common["peer_w_q"] = np.ascontiguousarray(inputs["peer_w_q"][0])
    sk = inputs["peer_sub_keys"][0]
    common["skT"] = np.ascontiguousarray(sk.reshape(16, 128, 128).transpose(2, 0, 1))
    U = inputs["peer_u"][0]
    common["peer_UT"] = np.ascontiguousarray(U.reshape(64, 256, 16, 128).transpose(0, 3, 2, 1))
    common["peer_v"] = np.ascontiguousarray(inputs["peer_v"][0])
    common["ple_norm_w"] = np.ascontiguousarray(inputs["ple_norm_w"][0:1])
    common["ple_w_gate"] = np.ascontiguousarray(inputs["ple_w_gate"][0])
    common["ple_w_proj"] = np.ascontiguousarray(inputs["ple_w_proj"][0])
    common["cmp_w1"] = np.ascontiguousarray(np.stack([inputs["cmp_k_w1"][0], inputs["cmp_v_w1"][0]]))
    common["cmp_w2"] = np.ascontiguousarray(np.stack([inputs["cmp_k_w2"][0], inputs["cmp_v_w2"][0]]))
    b1 = np.stack([inputs["cmp_k_b1"][0], inputs["cmp_v_b1"][0]])
    common["cmp_b1"] = np.ascontiguousarray(b1.reshape(2, 2, 128).transpose(2, 0, 1).reshape(128, 4))
    pos = np.stack([inputs["cmp_k_pos"][0], inputs["cmp_v_pos"][0]])
    posT = pos.transpose(2, 0, 1).reshape(64, 64)
    common["cmp_posT"] = np.ascontiguousarray(np.concatenate([posT, posT], axis=0))
    maps = []
    for b in range(8):
        m = dict(common)
        m["x"] = np.ascontiguousarray(inputs["x"][b])
        m["p"] = np.ascontiguousarray(inputs["p"][0, b])
        maps.append(m)
    return maps


def kernel(**inputs):
    inputs = {k_: np.asarray(v) for k_, v in inputs.items()}
    nc = build()
    maps = host_inputs(inputs)
    res = run_bass_kernel_spmd(nc, maps, core_ids=list(range(8)))
    out = np.stack([np.asarray(r["out"]) for r in res.results], axis=0)
    return out.astype(np.float32)
```

```python
import math
from contextlib import ExitStack
import numpy as np
import ml_dtypes
import concourse.bass as bass
import concourse.mybir as mybir
from concourse.bass_utils import run_bass_kernel_spmd

F32 = mybir.dt.float32
BF16 = mybir.dt.bfloat16
AF = mybir.ActivationFunctionType
ALU = mybir.AluOpType
AX = mybir.AxisListType

S = 2048
D = 2048
NT = S // 128
EPS = 1e-6
NEG = -30000.0


class Res:
    __slots__ = ("name", "w", "rs")

    def __init__(self, name):
        self.name = name
        self.w = None
        self.rs = {}


class Sched:
    EPOCH = 30000

    def __init__(self, nc, n_dma=24):
        self.nc = nc
        self.names = ["pe", "act", "dve", "pool", "sp"]
        self.ops = {e: [] for e in self.names}
        self.count = {e: 0 for e in self.names}
        self.epoch = {e: 0 for e in self.names}
        self.seen = {e: {} for e in self.names}
        self.n_dma = {"sp": n_dma, "pool": 12, "act": 4}
        self.dma_next = {q: 0 for q in self.n_dma}
        self.dma_uses = {q: [0] * n for q, n in self.n_dma.items()}
        self.keys = set()
        self.res = {}
        self.out_toks = []
        self.strict_war = True

    def R(self, *name):
        r = self.res.get(name)
        if r is None:
            r = Res(name)
            self.res[name] = r
        return r

    def op(self, e, fn, reads=(), writes=(), dma=False):
        waits = {}
        seen = self.seen[e]

        def need(tok, is_reader):
            if tok is None:
                return
            key, val, teng = tok
            if teng == e and (e == "pe" or (is_reader and not self.strict_war)):
                return
            if seen.get(key, 0) >= val:
                return
            if waits.get(key, 0) < val:
                waits[key] = val

        for r in reads:
            need(r.w, False)
        for w in writes:
            need(w.w, False)
            for tok in w.rs.values():
                need(tok, True)
        if dma:
            s = self.dma_next[e]
            self.dma_next[e] = (s + 1) % self.n_dma[e]
            self.dma_uses[e][s] += 1
            key = ("dma", e, s)
            val = 16 * self.dma_uses[e][s]
            if val > 16:
                need((key, val - 16, None), False)
            tok = (key, val, None)
            inc = (key, 16)
        else:
            if self.count[e] >= self.EPOCH:
                self.epoch[e] += 1
                self.count[e] = 0
            self.count[e] += 1
            key = (e, self.epoch[e])
            tok = (key, self.count[e], e)
            inc = (key, 1)
        self.keys.add(key)
        for k, v in waits.items():
            seen[k] = v
        self.ops[e].append((fn, list(waits.items()), inc))
        for r in reads:
            r.rs[tok[0]] = tok
        for w in writes:
            w.w = tok
            w.rs = {}
        return tok

    def barrier(self):
        toks = []
        for e in self.names:
            for ep in range(self.epoch[e] + 1):
                if (e, ep) in self.keys:
                    toks.append(((e, ep), self.count[e] if ep == self.epoch[e] else self.EPOCH))
        for q, n in self.n_dma.items():
            for s in range(n):
                if self.dma_uses[q][s]:
                    toks.append((("dma", q, s), 16 * self.dma_uses[q][s]))
        for e in self.names:
            seen = self.seen[e]
            waits = []
            for k, v in toks:
                if seen.get(k, 0) < v:
                    waits.append((k, v))
                    seen[k] = v
            if waits:
                self.ops[e].append((None, waits, None))

    def emit(self):
        nc = self.nc
        with ExitStack() as st:
            sems = {}
            for i, k in enumerate(sorted(self.keys, key=str)):
                sems[k] = st.enter_context(nc.semaphore("s%d" % i))
            block = st.enter_context(nc.Block())
            ops = self.ops

            def run(eng, lst):
                for fn, waits, inc in lst:
                    for k, v in waits:
                        eng.wait_ge(sems[k], v)
                    if fn is not None:
                        ins = fn(eng)
                        ins.then_inc(sems[inc[0]], inc[1])

            @block.tensor
            def _(eng):
                run(eng, ops["pe"])

            @block.scalar
            def _(eng):
                run(eng, ops["act"])

            @block.vector
            def _(eng):
                run(eng, ops["dve"])

            @block.gpsimd
            def _(eng):
                run(eng, ops["pool"])

            @block.sync
            def _(eng):
                run(eng, ops["sp"])


class K:
    def __init__(self, nc):
        self.nc = nc
        self.s = Sched(nc)
        self.banks = []
        self.bank_i = 0

    def mm(self, out, lhsT, rhs, start, stop, R, W, **kw):
        return self.s.op("pe", lambda e: e.matmul(out, lhsT, rhs, start=start, stop=stop, **kw), R, W)

    def tr(self, out, in_, ident, R, W):
        return self.s.op("pe", lambda e: e.transpose(out, in_, ident), R, W)

    def act(self, out, in_, func, R, W, **kw):
        return self.s.op("act", lambda e: e.activation(out, in_, func, **kw), R, W)

    def tt(self, out, in0, in1, op, R, W, eng="dve"):
        return self.s.op(eng, lambda e: e.tensor_tensor(out, in0, in1, op), R, W)

    def ts(self, out, in0, s1, s2, op0, op1, R, W, eng="dve", **kw):
        if s2 is None:
            return self.s.op(eng, lambda e: e.tensor_scalar(out, in0, s1, None, op0, **kw), R, W)
        return self.s.op(eng, lambda e: e.tensor_scalar(out, in0, s1, s2, op0, op1, **kw), R, W)

    def stt(self, out, in0, sc, in1, op0, op1, R, W):
        return self.s.op("dve", lambda e: e.scalar_tensor_tensor(out, in0, sc, in1, op0, op1), R, W)

    def cp(self, out, in_, R, W, eng="dve"):
        if eng == "act":
            return self.s.op("act", lambda e: e.copy(out, in_), R, W)
        return self.s.op(eng, lambda e: e.tensor_copy(out, in_), R, W)

    def red(self, out, in_, op, R, W, axis=AX.X):
        return self.s.op("dve", lambda e: e.tensor_reduce(out, in_, axis, op), R, W)

    def recip(self, out, in_, R, W):
        return self.s.op("dve", lambda e: e.reciprocal(out, in_), R, W)

    def max8(self, out, in_, R, W):
        return self.s.op("dve", lambda e: e.max(out, in_), R, W)

    def mrep(self, out, rep, vals, imm, R, W):
        return self.s.op("dve", lambda e: e.match_replace(out, rep, vals, imm), R, W)

    def memset(self, ap, val, W, eng="dve"):
        return self.s.op(eng, lambda e: e.memset(ap, val), (), W)

    def dma(self, out, in_, R, W, q="sp"):
        return self.s.op(q, lambda e: e.dma_start(out=out, in_=in_), R, W, dma=True)

    def bank(self):
        b = self.banks[self.bank_i]
        self.bank_i = (self.bank_i + 1) % len(self.banks)
        return b


def _perm_cols():
    u = list(range(0, 1024))
    q0 = 1024
    kc = list(range(2048, 2304))
    vc = list(range(2304, 2560))
    F = u + kc + vc
    qperm = []
    for c in range(8):
        for h in (c, c + 8):
            qperm += list(range(q0 + h * 64, q0 + (h + 1) * 64))

    def kperm(base):
        o = []
        for g in (0, 2, 1, 3):
            o += list(range(base + g * 64, base + (g + 1) * 64))
        return o

    ksl = kperm(2560)
    vsl = list(range(2816, 3072))
    kwn = kperm(3072)
    vwn = list(range(3328, 3584))
    gl = list(range(3584, 3632))
    T = qperm + ksl + kwn + vsl + vwn + gl
    return np.array(F + T, dtype=np.int64)


def t5_bucket_np(rel):
    rel = np.maximum(rel, 0)
    exact = 16
    logr = np.log(np.maximum(rel, 1).astype(np.float32) / np.float32(exact))
    large = exact + (logr / np.float32(math.log(128 / exact)) * (32 - exact)).astype(np.int32)
    return np.where(rel < exact, rel, np.minimum(large, 31))


def build(debug=()):
    nc = bass.Bass("TRN2", target_bir_lowering=False)
    k = K(nc)
    s = k.s
    dbg = set(debug)

    def din(name, shape, dt=F32):
        return nc.dram_tensor(name, list(shape), dt, kind="ExternalInput").ap()

    def dscr(name, shape, dt):
        kind = "ExternalOutput" if name in dbg else "Internal"
        return nc.dram_tensor(name, list(shape), dt, kind=kind).ap()

    x_d = din("x", [S, D])
    p_d = din("p", [S, 256])
    wF_d = din("w_inF", [12, 128, 16, 128])
    wT_d = din("w_inT", [D, 2096])
    mixw_d = din("mix_norm_w", [1, D])
    qw_d = din("q_norm_w", [1, 64])
    kw_d = din("k_norm_w", [1, 64])
    poolw_d = din("pool_w", [4, 256, 256])
    pools_d = din("pool_scale", [128, 8])
    rc_d = din("rc16", [1, 64])
    ident_d = din("ident", [128, 128])
    out_d = nc.dram_tensor("out", [S, D], F32, kind="ExternalOutput").ap()
    w1_d = din("cmp_w1", [2, 2048, 256])
    b1_d = din("cmp_b1", [128, 4])
    w2_d = din("cmp_w2", [2, 256, 64])
    posT_d = din("cmp_posT", [128, 64])
    ovl_d = din("ovl", [128, 32])
    E2_d = din("E2", [128, S])
    biasg_d = din("bias_g", [128, 6, 2048])
    maskc_d = din("mask_c", [128, 6, 2048])
    biasCg_d = din("biasC_g", [128, 2, 2048])
    maskCc_d = din("maskC_c", [128, 2, 2048])
    cA_d = din("cA", [128, 512])
    cB_d = din("cB", [128, 512])
    wout_d = din("w_out", [D, D])
    ffnw_d = din("ffn_norm_w", [1, D])
    wq_d = din("peer_w_q", [D, D])
    skT_d = din("skT", [128, 16, 128])
    UT_d = din("peer_UT", [64, 128, 16, 256])
    V_d = din("peer_v", [16384, D])
    plew_d = din("ple_norm_w", [1, D])
    wg_d = din("ple_w_gate", [D, D])
    wp_d = din("ple_w_proj", [256, D])
    x1_d = dscr("x1_s", [S, D], F32)
    x2_d = dscr("x2_s", [S, D], F32)
    xn2T_d = dscr("xn2T_s", [128, 16, S], BF16)
    ab_d = dscr("ab_s", [S, 2048], BF16)
    thz_d = dscr("thz_s", [S, 16], F32)
    kcT_d = dscr("kcT_s", [128, 2, 128], BF16)
    vcmp_d = dscr("vcmp_s", [128, 4, 65], BF16)

    qT_d = dscr("qT_s", [128, 8, S], BF16)
    kT_d = dscr("kT_s", [128, 4, S], BF16)
    v_d = dscr("v_s", [S, 520], BF16)
    gates_d = dscr("gates_s", [S, 48], F32)
    kvcT_d = dscr("kvcT_s", [128, 4, S], BF16)
    catT_d = dscr("catT_s", [128, 16, S], BF16)

    with ExitStack() as top:
        def sb(name, shape, dt):
            return top.enter_context(nc.sbuf_tensor(name, list(shape), dt))

        for i in range(8):
            t = top.enter_context(nc.psum_tensor("bank%d" % i, [128, 512], F32))
            k.banks.append((t, s.R("bank", i)))

        ident_f = sb("ident_f", [128, 128], F32)
        ident_b = sb("ident_b", [128, 128], BF16)
        r_id = s.R("ident")
        k.dma(ident_f[:], ident_d, (), (r_id,))
        k.cp(ident_b[:], ident_f[:], (r_id,), (r_id,))

        with ExitStack() as ph:
            def sb1(name, shape, dt):
                return ph.enter_context(nc.sbuf_tensor(name, list(shape), dt))

            w_sb = sb1("w_in_sb", [128, 16, 2096], BF16)
            r_w = s.R("w_in")
            w_view = wT_d.rearrange("(k p) n -> p k n", p=128)
            for kk in range(0, 16, 2):
                k.dma(w_sb[:, kk:kk + 2, :], w_view[:, kk:kk + 2, :], (), (r_w,), q="pool")
            wF_sb = [sb1("wF_sb%d" % i, [128, 16, 128], BF16) for i in range(2)]
            r_wF = [s.R("wF", i) for i in range(2)]
            mixw_sb = sb1("mixw_sb", [128, D], F32)
            r_c = s.R("consts1")
            k.dma(mixw_sb[:], mixw_d.to_broadcast([128, D]), (), (r_c,))
            qw_sb = sb1("qw_sb", [128, 64], F32)
            kw_sb = sb1("kw_sb", [128, 64], F32)
            k.dma(qw_sb[:], qw_d.to_broadcast([128, 64]), (), (r_c,))
            k.dma(kw_sb[:], kw_d.to_broadcast([128, 64]), (), (r_c,))
            rc_sb = sb1("rc_sb", [128, 64], F32)
            k.dma(rc_sb[:], rc_d.to_broadcast([128, 64]), (), (r_c,))
            pools_sb = sb1("pools_sb", [128, 8], F32)
            k.dma(pools_sb[:], pools_d, (), (r_c,))
            poolw_sb = sb1("poolw_sb", [128, 4, 2, 256], BF16)
            k.dma(poolw_sb[:], poolw_d.rearrange("g (kk p) d -> p g kk d", p=128), (), (r_c,), q="pool")

            xt = [sb1("xt%d" % i, [128, D], F32) for i in range(2)]
            r_xt = [s.R("xt", i) for i in range(2)]
            xn = [sb1("xn%d" % i, [128, D], BF16) for i in range(2)]
            r_xn = [s.R("xn", i) for i in range(2)]
            stat = [sb1("stat%d" % i, [128, 4], F32) for i in range(2)]
            r_stat = [s.R("stat", i) for i in range(2)]
            hnT = sb1("hnT", [128, 16, 512], BF16)
            r_hnT = [s.R("hnT", j) for j in range(4)]
            uT = sb1("uT", [128, 8, 528], F32)
            r_uT = s.R("uT")
            pa = sb1("pool_a", [128, 2, 528], F32)
            pb = sb1("pool_b", [128, 2, 528], F32)
            r_pa, r_pb = s.R("pa"), s.R("pb")
            ptmp = sb1("pool_tmp", [128, 2, 16], F32)
            r_ptmp = s.R("ptmp")
            mixT = sb1("mixT", [128, 8, 512], BF16)
            r_mixT = [s.R("mixT", g) for g in range(4)]
            yT = sb1("yT", [128, 8, 512], BF16)
            r_yT = s.R("yT")
            kvc_st = sb1("kvc_st", [128, 4, 512], BF16)
            r_kvc = s.R("kvc_st")
            qT_st = sb1("qT_st", [128, 8, 512], BF16)
            r_qT = s.R("qT_st")
            kT_st = sb1("kT_st", [128, 4, 512], BF16)
            r_kT = s.R("kT_st")
            v_st = [sb1("v_st%d" % i, [128, 8, 65], BF16) for i in range(2)]
            r_vst = [s.R("v_st", i) for i in range(2)]
            for i in range(2):
                k.memset(v_st[i][:, :, 64:65], 1.0, (r_vst[i],))
            g_st = [sb1("g_st%d" % i, [128, 48], F32) for i in range(2)]
            r_gst = [s.R("g_st", i) for i in range(2)]
            sq = sb1("sq", [128, 1024], F32)
            r_sq = s.R("sq")
            junk = sq[:].bitcast(BF16)
            r_junk = r_sq
            qn32 = sb1("qn32", [128, 1024], F32)
            r_qn32 = s.R("qn32")
            qnb = sb1("qnb", [128, 1536], BF16)
            r_qnb = s.R("qnb")
            ss24 = sb1("ss24", [128, 24], F32)
            rs24 = sb1("rs24", [128, 24], F32)
            r_ss = s.R("ss24")

            k.memset(uT[:, :, 0:16], 0.0, (r_uT,))

            for st_i in range(4):
                for j in range(4):
                    tt_i = st_i * 4 + j
                    sl = tt_i % 2
                    k.dma(xt[sl][:], x_d[tt_i * 128:(tt_i + 1) * 128, :], (), (r_xt[sl],))
                    k.act(junk, xt[sl][:], AF.Square, (r_xt[sl],), (r_junk, r_stat[sl]),
                          accum_out=stat[sl][:, 0:1])
                    k.act(stat[sl][:, 1:2], stat[sl][:, 0:1], AF.Sqrt, (r_stat[sl],), (r_stat[sl],),
                          scale=1.0 / D, bias=EPS)
                    k.recip(stat[sl][:, 2:3], stat[sl][:, 1:2], (r_stat[sl],), (r_stat[sl],))
                    k.stt(xn[sl][:], xt[sl][:], stat[sl][:, 2:3], mixw_sb[:], ALU.mult, ALU.mult,
                          (r_xt[sl], r_stat[sl], r_c), (r_xn[sl],))
                    for half in range(2):
                        bt, br = k.bank()
                        bv = bt[:].bitcast(BF16)
                        for c in range(8):
                            kk = half * 8 + c
                            k.tr(bv[:, c * 128:(c + 1) * 128], xn[sl][:, kk * 128:(kk + 1) * 128], ident_b[:],
                                 (r_xn[sl], r_id), (br,))
                        dst = hnT[:, half * 8:(half + 1) * 8, j * 128:(j + 1) * 128]
                        src = bv.rearrange("p (c t) -> p c t", c=8)
                        k.cp(dst, src, (br,), (r_hnT[j],), eng=("act" if half == 0 else "dve"))
                for fc in range(12):
                    wi = fc % 2
                    k.dma(wF_sb[wi][:], wF_d[fc], (), (r_wF[wi],), q="pool")
                    bt, br = k.bank()
                    for kk in range(16):
                        k.mm(bt[:], wF_sb[wi][:, kk, :], hnT[:, kk, :], kk == 0, kk == 15,
                             (r_wF[wi],) + tuple(r_hnT), (br,))
                    if fc < 8:
                        k.cp(uT[:, fc, 16:528], bt[:], (br,), (r_uT,), eng="act")
                    else:
                        k.cp(kvc_st[:, fc - 8, :], bt[:], (br,), (r_kvc,), eng="act")
                k.dma(kvcT_d[:, :, st_i * 512:(st_i + 1) * 512], kvc_st[:], (r_kvc,), (s.R("kvcT_d", st_i),))
                for g in range(4):
                    w = 2 << g
                    cur = uT[:, 2 * g:2 * g + 2, :]
                    r_cur = r_uT
                    step = 1
                    bufs = [(pa, r_pa), (pb, r_pb)]
                    bi = 0
                    while step < w:
                        lo = 2 * step - 1
                        nb, r_nb = bufs[bi]
                        bi ^= 1
                        k.tt(nb[:, :, lo:528], cur[:, :, lo:528], cur[:, :, lo - step:528 - step], ALU.add,
                             (r_cur,), (r_nb,))
                        cur, r_cur = nb, r_nb
                        step *= 2
                    k.stt(mixT[:, 2 * g:2 * g + 2, :], cur[:, :, 16:528], 1.0 / w, uT[:, 2 * g:2 * g + 2, 16:528],
                          ALU.mult, ALU.subtract, (r_cur, r_uT), (r_mixT[g],))
                    if st_i == 0:
                        k.tt(ptmp[:], cur[:, :, 16:32],
                             rc_sb[:, g * 16:(g + 1) * 16].unsqueeze(1).to_broadcast([128, 2, 16]), ALU.mult,
                             (r_cur, r_c), (r_ptmp,))
                        k.tt(mixT[:, 2 * g:2 * g + 2, 0:16], ptmp[:], uT[:, 2 * g:2 * g + 2, 16:32], ALU.subtract,
                             (r_ptmp, r_uT), (r_mixT[g],))
                k.cp(uT[:, :, 0:16], uT[:, :, 512:528], (r_uT,) + tuple(r_mixT), (r_uT,), eng="pool")
                for oc in range(8):
                    g = oc // 2
                    hf = oc % 2
                    bt, br = k.bank()
                    for kk in range(2):
                        k.mm(bt[:], poolw_sb[:, g, kk, hf * 128:(hf + 1) * 128], mixT[:, 2 * g + kk, :],
                             kk == 0, kk == 1, (r_c, r_mixT[g]), (br,))
                    k.act(yT[:, oc, :], bt[:], AF.Copy, (br, r_c), (r_yT,), scale=pools_sb[:, oc:oc + 1])
                k.dma(catT_d[:, 0:8, st_i * 512:(st_i + 1) * 512], yT[:], (r_yT,), (s.R("catT_d", st_i),))
                for j in range(4):
                    tt_i = st_i * 4 + j
                    sl = tt_i % 2
                    T0 = 0
                    segs = [(T0, 512), (T0 + 512, 512), (T0 + 1024, 512), (T0 + 1536, 512), (T0 + 2048, 48)]
                    bks = []
                    for (c0, n) in segs:
                        bt, br = k.bank()
                        for kk in range(16):
                            k.mm(bt[:, 0:n], hnT[:, kk, j * 128:(j + 1) * 128], w_sb[:, kk, c0:c0 + n],
                                 kk == 0, kk == 15, (r_w, r_hnT[j]), (br,))
                        bks.append((bt, br))
                    for bi_, (bt, br) in enumerate(bks[0:3]):
                        k.act(sq[:, 0:512], bt[:], AF.Square, (br,), (r_sq,))
                        k.red(ss24[:, bi_ * 8:(bi_ + 1) * 8], sq[:, 0:512].rearrange("p (h d) -> p h d", d=64), ALU.add,
                              (r_sq,), (r_ss,))
                    k.act(rs24[:], ss24[:], AF.Sqrt, (r_ss,), (r_ss,), scale=1.0 / 64, bias=EPS)
                    k.recip(rs24[:], rs24[:], (r_ss,), (r_ss,))
                    for bi_, (bt, br) in enumerate(bks[0:3]):
                        k.tt(qn32[:, 0:512].rearrange("p (h d) -> p h d", d=64),
                             bt[:].rearrange("p (h d) -> p h d", d=64),
                             rs24[:, bi_ * 8:(bi_ + 1) * 8].unsqueeze(2).to_broadcast([128, 8, 64]), ALU.mult,
                             (br, r_ss), (r_qn32,))
                        wsb = qw_sb if bi_ < 2 else kw_sb
                        k.stt(qnb[:, bi_ * 512:(bi_ + 1) * 512].rearrange("p (h d) -> p h d", d=64),
                              qn32[:, 0:512].rearrange("p (h d) -> p h d", d=64),
                              0.125 if bi_ < 2 else 1.0,
                              wsb[:].unsqueeze(1).to_broadcast([128, 8, 64]), ALU.mult, ALU.mult,
                              (r_qn32, r_c), (r_qnb,))
                    bt, br = k.bank()
                    bv = bt[:].bitcast(BF16)
                    for c in range(8):
                        k.tr(bv[:, c * 128:(c + 1) * 128], qnb[:, c * 128:(c + 1) * 128], ident_b[:], (r_qnb, r_id), (br,))
                    k.cp(qT_st[:, :, j * 128:(j + 1) * 128], bv.rearrange("p (c t) -> p c t", c=8), (br,), (r_qT,),
                         eng="act")
                    bt, br = k.bank()
                    bv = bt[:].bitcast(BF16)
                    for c in range(4):
                        k.tr(bv[:, c * 128:(c + 1) * 128], qnb[:, 1024 + c * 128:1024 + (c + 1) * 128], ident_b[:],
                             (r_qnb, r_id), (br,))
                    k.cp(kT_st[:, :, j * 128:(j + 1) * 128], bv[:, 0:512].rearrange("p (c t) -> p c t", c=4), (br,),
                         (r_kT,), eng="act")
                    bt, br = bks[3]
                    k.cp(v_st[sl][:, :, 0:64], bt[:].rearrange("p (a d) -> p a d", d=64), (br,), (r_vst[sl],), eng="act")
                    k.dma(v_d[tt_i * 128:(tt_i + 1) * 128, :], v_st[sl][:].rearrange("p a d -> p (a d)"), (r_vst[sl],),
                          (s.R("v_d", tt_i),))
                    bt, br = bks[4]
                    k.act(g_st[sl][:], bt[:, 0:48], AF.Sigmoid, (br,), (r_gst[sl],))
                    k.dma(gates_d[tt_i * 128:(tt_i + 1) * 128, :], g_st[sl][:], (r_gst[sl],), (s.R("g_d", tt_i),))
                k.dma(qT_d[:, :, st_i * 512:(st_i + 1) * 512], qT_st[:], (r_qT,), (s.R("qT_d", st_i),))
                k.dma(kT_d[:, :, st_i * 512:(st_i + 1) * 512], kT_st[:], (r_kT,), (s.R("kT_d", st_i),))
        s.barrier()

        all_banks = list(k.banks)
        with ExitStack() as ph3:
            def sb3(name, shape, dt):
                return ph3.enter_context(nc.sbuf_tensor(name, list(shape), dt))

            kcmpT = sb3("kcmpT", [128, 2, 128], BF16)
            r_kcT = s.R("kcmpT")
            vcmp = sb3("vcmp", [128, 4, 65], BF16)
            r_vc = s.R("vcmp")
            kw_sb2 = sb3("kw_sb2", [128, 64], F32)
            r_c3 = s.R("consts3")
            k.dma(kw_sb2[:], kw_d.to_broadcast([128, 64]), (), (r_c3,))
            btab = sb3("btab", [128, 6, 2048], F32)
            bC = sb3("bC", [128, 2, 2048], F32)
            mtmp = [sb3("mtmp%d" % i, [128, 2048], F32) for i in range(2)]
            r_mt = [s.R("mtmp", i) for i in range(2)]
            r_bt = s.R("btab")
            k.dma(btab[:], biasg_d, (), (r_bt,))
            k.dma(bC[:], biasCg_d, (), (r_bt,))
            for i in range(8):
                src_m = maskc_d[:, i, :] if i < 6 else maskCc_d[:, i - 6, :]
                dst_b = btab[:, i, :] if i < 6 else bC[:, i - 6, :]
                k.dma(mtmp[i % 2][:], src_m, (), (r_mt[i % 2],))
                k.tt(dst_b, dst_b, mtmp[i % 2][:], ALU.add, (r_mt[i % 2], r_bt), (r_bt,))
            E2 = sb3("E2_sb", [128, S], BF16)
            k.dma(E2[:], E2_d, (), (r_c3,), q="pool")
            ovl = sb3("ovl_sb", [128, 32], F32)
            cA = sb3("cA_sb", [128, 512], F32)
            cB = sb3("cB_sb", [128, 512], F32)
            k.dma(ovl[:], ovl_d, (), (r_c3,))
            k.dma(cA[:], cA_d, (), (r_c3,))
            k.dma(cB[:], cB_d, (), (r_c3,))
            with ExitStack() as ph2:
                def sb2(name, shape, dt):
                    return ph2.enter_context(nc.sbuf_tensor(name, list(shape), dt))

                kvc = sb2("kvc", [128, 4, S], BF16)
                r_kvc2 = s.R("kvc2")
                k.dma(kvc[:], kvcT_d, (), (r_kvc2,))
                posT = sb2("posT", [128, 64], F32)
                b1 = sb2("b1", [128, 4], F32)
                r_c2 = s.R("consts2")
                k.dma(posT[:], posT_d, (), (r_c2,))
                k.dma(b1[:], b1_d, (), (r_c2,))
                w1 = sb2("w1", [128, 2, 32, 256], BF16)
                w2 = sb2("w2", [128, 2, 2, 64], BF16)
                r_w1 = s.R("w1")
                for kv in range(2):
                    for hb in range(2):
                        k.dma(w1[hb * 64:(hb + 1) * 64, kv], w1_d[kv].rearrange("(j d) c -> d j c", d=64), (), (r_w1,),
                              q="pool")
                    k.dma(w2[:, kv], w2_d[kv].rearrange("(h p) d -> p h d", p=128), (), (r_w1,), q="pool")
                kA = sb2("kA", [128, 4, S], BF16)
                kB = sb2("kB", [128, 4, S], BF16)
                r_kAB = s.R("kAB")
                for c in range(4):
                    kv = c // 2
                    for (dst, o) in ((kA, 0), (kB, 16)):
                        k.tt(dst[:, c, :].rearrange("p (i s) -> p i s", s=16),
                             kvc[:, c, :].rearrange("p (i s) -> p i s", s=16),
                             posT[:, kv * 32 + o:kv * 32 + o + 16].unsqueeze(1).to_broadcast([128, 128, 16]), ALU.add,
                             (r_kvc2, r_c2), (r_kAB,))
                hT = [sb2("hT%d" % i, [128, 2, 128], BF16) for i in range(2)]
                r_hT = [s.R("hT", i) for i in range(2)]
                knb = sb2("knb", [128, 2, 128], BF16)
                r_knb = s.R("knb")
                k.memset(knb[:], 0.0, (r_knb,))
                k.memset(vcmp[:], 0.0, (r_vc,))
                k.memset(vcmp[:, :, 64:65], 1.0, (r_vc,))
                st2 = sb2("st2", [128, 4], F32)
                junk64 = sb2("junk64", [128, 64], F32)
                r_st2 = s.R("st2")
                it = 0
                for kv in range(2):
                    for g in range(4):
                        chunk = 2 * kv + g // 2
                        base = 64 * (g % 2)
                        hi = it % 2
                        it += 1
                        for half in range(2):
                            bt, br = k.bank()
                            for j in range(32):
                                srcT = kA if j < 16 else kB
                                view = srcT[base:base + 64, chunk, :].rearrange("p (i s) -> p i s", s=16)
                                rhs = view[:, 0:127, j] if j < 16 else view[:, 1:128, j - 16]
                                k.mm(bt[:, 0:127], w1[base:base + 64, kv, j, half * 128:(half + 1) * 128], rhs,
                                     j == 0, j == 31, (r_w1, r_kAB), (br,))
                            k.act(hT[hi][:, half, 0:127], bt[:, 0:127], AF.Gelu, (br, r_c2), (r_hT[hi],),
                                  bias=b1[:, kv * 2 + half:kv * 2 + half + 1])
                        bt, br = k.bank()
                        for half in range(2):
                            k.mm(bt[0:127, 0:64], hT[hi][:, half, 0:127], w2[:, kv, half, :], half == 0, half == 1,
                                 (r_hT[hi], r_w1), (br,))
                        if kv == 0:
                            k.act(junk64[0:127, :], bt[0:127, 0:64], AF.Square, (br,), (r_st2,),
                                  accum_out=st2[0:127, 0:1])
                            k.act(st2[0:127, 1:2], st2[0:127, 0:1], AF.Sqrt, (r_st2,), (r_st2,), scale=1.0 / 64, bias=EPS)
                            k.recip(st2[0:127, 2:3], st2[0:127, 1:2], (r_st2,), (r_st2,))
                            k.stt(knb[0:127, g % 2, 64 * (g // 2):64 * (g // 2) + 64], bt[0:127, 0:64], st2[0:127, 2:3],
                                  kw_sb2[0:127, :], ALU.mult, ALU.mult, (br, r_st2, r_c3), (r_knb,))
                        else:
                            k.cp(vcmp[0:127, g, 0:64], bt[0:127, 0:64], (br,), (r_vc,), eng="act")
                bt, br = k.bank()
                bv = bt[:].bitcast(BF16)
                for c in range(2):
                    k.tr(bv[:, c * 128:(c + 1) * 128], knb[:, c, :], ident_b[:], (r_knb, r_id), (br,))
                k.cp(kcmpT[:], bv[:, 0:256].rearrange("p (c t) -> p c t", c=2), (br,), (r_kcT,), eng="act")
                if "kcT_s" in dbg:
                    k.dma(kcT_d, kcmpT[:], (r_kcT,), (s.R("kcT_d"),))
                    k.dma(vcmp_d, vcmp[:], (r_vc,), (s.R("vcmp_d"),))
                s.barrier()
            qT = sb3("qT", [128, 8, S], BF16)
            kT = sb3("kT", [128, 4, S], BF16)
            v_all = sb3("v_all", [128, 16, 520], BF16)
            gates = sb3("gates", [128, 16, 48], F32)
            r_in3 = s.R("in3")
            k.dma(qT[:], qT_d, (), (r_in3,))
            k.dma(kT[:], kT_d, (), (r_in3,))
            k.dma(v_all[:], v_d.rearrange("(t p) n -> p t n", p=128), (), (r_in3,))
            k.dma(gates[:], gates_d.rearrange("(t p) n -> p t n", p=128), (), (r_in3,))
            pen_pad = [sb3("pen_pad%d" % i, [128, 128], BF16) for i in range(2)]
            r_pp = [s.R("pen_pad", i) for i in range(2)]
            for i in range(2):
                k.memset(pen_pad[i][:], 0.0, (r_pp[i],))
            pen2 = sb3("pen2", [128, 512], BF16)
            r_pen2 = s.R("pen2")
            k.memset(pen2[:], 0.0, (r_pen2,))
            NB = 4
            sc = [sb3("sc%d" % i, [128, 512], F32) for i in range(NB)]
            r_sc = [s.R("sc", i) for i in range(NB)]
            pT = [sb3("pT%d" % i, [128, 512], BF16) for i in range(NB)]
            r_pT = [s.R("pT", i) for i in range(NB)]
            pc = sb3("pc", [128, 512], F32)
            pn = sb3("pn", [128, 512], F32)
            pcs = sb3("pcs", [128, 128], F32)
            pcsT = sb3("pcsT", [128, 128], F32)
            pcTb = sb3("pcTb", [128, 512], BF16)
            r_pc, r_pn, r_pcs, r_pcsT, r_pcTb = s.R("pc"), s.R("pn"), s.R("pcs"), s.R("pcsT"), s.R("pcTb")
            impb = sb3("impb", [128, 32], F32)
            m8 = sb3("m8", [128, 8], F32)
            zst = sb3("zst", [128, 20], F32)
            r_imp, r_z = s.R("impb"), s.R("zst")
            oacc = sb3("oacc", [128, 256], F32)
            otmp = sb3("otmp", [128, 256], F32)
            r_oacc, r_otmp = s.R("oacc"), s.R("otmp")
            o_tile = [sb3("o_tile%d" % i, [128, 1024], BF16) for i in range(2)]
            r_ot = [s.R("o_tile", i) for i in range(2)]
            aT_st = [sb3("aT_st%d" % i, [128, 8, 128], BF16) for i in range(2)]
            r_aT = [s.R("aT_st", i) for i in range(2)]

            k.banks = all_banks[6:8]
            k.bank_i = 0
            fixed = all_banks[0:6]
            pen2b = [pen2, sb3("pen2_b", [128, 512], BF16)]
            r_pen2b = [r_pen2, s.R("pen2b")]
            k.memset(pen2b[1][:], 0.0, (r_pen2b[1],))
            r_zA, r_zE = s.R("zstA"), s.R("zstE")
            wi_box = [0]

            def make_iter(qi, g, itn):
                q0 = qi * 128
                base = 64 * (g // 2)
                cq = 4 * (g % 2)
                ck = g % 2
                ob = fixed[0:3] if itn % 2 == 0 else fixed[3:6]
                (oc_t, oc_r), (os_t, os_r), (ow_t, ow_r) = ob
                qrhs = qT[base:base + 64, cq:cq + 4, q0:q0 + 128]
                p2 = pen2b[itn % 2]
                r_p2 = r_pen2b[itn % 2]
                pi = g // 2
                osl = qi % 2

                def gen_chain():
                    bt, br = k.bank()
                    for r in range(4):
                        k.mm(bt[:, r * 128:(r + 1) * 128], qT[base:base + 64, cq + r, q0:q0 + 128],
                             kcmpT[base:base + 64, ck, :], True, True, (r_in3, r_kcT), (br,))
                    j0 = 128 - 8 * qi
                    bCv = bC[:].rearrange("p a (h j) -> p (a h) j", j=256)[:, 4 * g:4 * g + 4, j0:j0 + 128]
                    k.tt(pn[:].rearrange("p (r c) -> p r c", c=128), bt[:].rearrange("p (r c) -> p r c", c=128), bCv,
                         ALU.add, (br, r_bt), (r_pn,))
                    yield
                    for r in range(4):
                        k.act(pc[:, r * 128:(r + 1) * 128], pn[:, r * 128:(r + 1) * 128], AF.Exp, (r_pn,), (r_pc, r_zA),
                              accum_out=zst[:, r:r + 1])
                    yield
                    k.ts(zst[:, 4:8], zst[:, 0:4], 1e-30, None, ALU.max, None, (r_zA,), (r_zA,))
                    k.recip(zst[:, 4:8], zst[:, 4:8], (r_zA,), (r_zA,))
                    bt, br = k.bank()
                    for r in range(4):
                        k.tr(bt[:, r * 128:(r + 1) * 128], pc[:, r * 128:(r + 1) * 128], ident_f[:], (r_pc, r_id), (br,))
                    k.cp(pcTb[:], bt[:], (br,), (r_pcTb,), eng="act")
                    yield
                    for r in range(4):
                        k.mm(oc_t[:, r * 65:(r + 1) * 65], pcTb[:, r * 128:(r + 1) * 128], vcmp[:, g, :], True, True,
                             (r_pcTb, r_vc), (oc_r,))
                    k.tt(pn[:].rearrange("p (r c) -> p r c", c=128), pc[:].rearrange("p (r c) -> p r c", c=128),
                         zst[:, 4:8].unsqueeze(2).to_broadcast([128, 4, 128]), ALU.mult, (r_pc, r_zA), (r_pn,))
                    yield
                    k.red(pcs[:], pn[:].rearrange("p (r c) -> p c r", c=128), ALU.add, (r_pn,), (r_pcs,))
                    yield
                    bt, br = k.bank()
                    k.tr(bt[:, 0:128], pcs[:], ident_f[:], (r_pcs, r_id), (br,))
                    k.cp(pcsT[:], bt[:, 0:128], (br,), (r_pcsT,), eng="act")
                    yield
                    bt, br = k.bank()
                    k.mm(bt[:, 0:32], pcsT[:], ovl[:], True, True, (r_pcsT, r_c3), (br,))
                    k.tt(impb[:], bt[:, 0:32], cA[:, qi * 32:(qi + 1) * 32], ALU.mult, (br, r_c3), (r_imp,))
                    yield
                    k.tt(impb[:], impb[:], cB[:, qi * 32:(qi + 1) * 32], ALU.add, (r_imp, r_c3), (r_imp,))
                    yield
                    k.max8(m8[:], impb[:], (r_imp,), (r_imp,))
                    yield
                    k.ts(pen_pad[pi][:, base:base + 32], impb[:], m8[:, 7:8], NEG, ALU.is_lt, ALU.mult,
                         (r_imp,), (r_pp[pi],))
                    yield
                    bt, br = k.bank()
                    bv = bt[:].bitcast(BF16)
                    k.tr(bv[:, 0:128], pen_pad[pi][:], ident_b[:], (r_pp[pi], r_id), (br,))
                    k.cp(p2[base:base + 64, :].rearrange("p (r q) -> p r q", q=128),
                         bv[base:base + 64, 0:128].unsqueeze(1).to_broadcast([64, 4, 128]), (br,), (r_p2,))
                    yield

                first = {"s": True, "w": True}
                last_kc = {"s": qi, "w": qi}

                def emit_pv(job):
                    brn, kc, idx, wsl = job
                    o_t, o_r = (os_t, os_r) if brn == "s" else (ow_t, ow_r)
                    col0 = (g if brn == "s" else 4 + g) * 65
                    for r in range(4):
                        k.mm(o_t[:, r * 65:(r + 1) * 65], pT[wsl][:, r * 128:(r + 1) * 128],
                             v_all[:, kc, col0:col0 + 65],
                             first[brn] and r == 0, kc == last_kc[brn], (r_pT[wsl], r_in3), (o_r,),
                             skip_group_check=True)
                    first[brn] = False

                def gen_jobs():
                    jobs = [("w", qi + off, -off) for off in range(max(-4, -qi), 1)]
                    for kc in range(0, qi + 1):
                        off = kc - qi
                        jobs.append(("s", kc, 0 if off == 0 else (1 if off == -1 else 5)))
                    prev_job = None
                    for (brn, kc, idx) in jobs:
                        bt, br = k.bank()
                        kch = ck if brn == "s" else 2 + ck
                        need_pen = brn == "s" and kc != qi and qi >= 4
                        k.mm(bt[:], kT[base:base + 64, kch, kc * 128:(kc + 1) * 128], qrhs, True, not need_pen,
                             (r_in3,), (br,))
                        if need_pen:
                            k.mm(bt[:], E2[base:base + 64, kc * 128:(kc + 1) * 128], p2[base:base + 64, :], False,
                                 True, (r_c3, r_p2), (br,))
                        if prev_job is not None:
                            emit_pv(prev_job)
                        wsl = wi_box[0] % NB
                        wi_box[0] += 1
                        k.tt(sc[wsl][:], bt[:], btab[:, idx, g * 512:(g + 1) * 512], ALU.add, (br, r_bt),
                             (r_sc[wsl],))
                        k.act(pT[wsl][:], sc[wsl][:], AF.Exp, (r_sc[wsl],), (r_pT[wsl],))
                        prev_job = (brn, kc, idx, wsl)
                        yield
                    if prev_job is not None:
                        emit_pv(prev_job)

                def gen_combine():
                    for bi_, (o_t, o_r) in enumerate(((oc_t, oc_r), (os_t, os_r), (ow_t, ow_r))):
                        ov = o_t[:, 0:260].rearrange("p (r e) -> p r e", e=65)
                        zz = zst[:, 8 + 4 * bi_:12 + 4 * bi_]
                        k.ts(zz, ov[:, :, 64], 1e-30, None, ALU.max, None, (o_r,), (r_zE,))
                        yield
                        k.recip(zz, zz, (r_zE,), (r_zE,))
                        yield
                        gv = gates[:, qi, :].rearrange("p (h b) -> p h b", b=3)[:, 4 * g:4 * g + 4, bi_]
                        k.tt(zz, zz, gv, ALU.mult, (r_zE, r_in3), (r_zE,))
                        yield
                        zb = zz.unsqueeze(2).to_broadcast([128, 4, 64])
                        if bi_ == 0:
                            k.tt(oacc[:].rearrange("p (r d) -> p r d", d=64), ov[:, :, 0:64], zb, ALU.mult,
                                 (o_r, r_zE), (r_oacc,))
                        else:
                            k.tt(otmp[:].rearrange("p (r d) -> p r d", d=64), ov[:, :, 0:64], zb, ALU.mult,
                                 (o_r, r_zE), (r_otmp,))
                            yield
                            k.tt(oacc[:], oacc[:], otmp[:], ALU.add, (r_oacc, r_otmp), (r_oacc,))
                        yield
                    k.cp(o_tile[osl][:, g * 256:(g + 1) * 256], oacc[:], (r_oacc,), (r_ot[osl],))
                    yield
                    if g == 3:
                        bt, br = k.bank()
                        bv = bt[:].bitcast(BF16)
                        for c in range(8):
                            k.tr(bv[:, c * 128:(c + 1) * 128], o_tile[osl][:, c * 128:(c + 1) * 128], ident_b[:],
                                 (r_ot[osl], r_id), (br,))
                        k.cp(aT_st[osl][:], bv.rearrange("p (c t) -> p c t", c=8), (br,), (r_aT[osl],), eng="act")
                        k.dma(catT_d[:, 8:16, q0:q0 + 128], aT_st[osl][:], (r_aT[osl],), (s.R("catT_a", qi),))
                        yield

                return gen_chain, gen_jobs, gen_combine

            all_it = [(qi, g) for qi in range(NT) for g in range(4)]
            objs = [make_iter(qi, g, n) for n, (qi, g) in enumerate(all_it)]
            N_IT = len(objs)
            for _ in objs[0][0]():
                pass
            for n in range(N_IT):
                queue = []
                if n >= 1:
                    queue.append(objs[n - 1][2]())
                if n + 1 < N_IT:
                    queue.append(objs[n + 1][0]())

                def qstep():
                    while queue:
                        try:
                            next(queue[0])
                            return
                        except StopIteration:
                            queue.pop(0)

                for _ in objs[n][1]():
                    qstep()
                while queue:
                    qstep()
            for _ in objs[N_IT - 1][2]():
                pass
            k.banks = all_banks
            k.bank_i = 0
            s.barrier()
        def norm_tile(c_, src_t, r_src, stat_t, r_stat_, junk_ap, r_junk_, wb, r_wb, dst_bf, r_dst):
            k.act(junk_ap, src_t, AF.Square, (r_src,), (r_junk_, r_stat_), accum_out=stat_t[:, 0:1])
            k.act(stat_t[:, 1:2], stat_t[:, 0:1], AF.Sqrt, (r_stat_,), (r_stat_,), scale=1.0 / D, bias=EPS)
            k.recip(stat_t[:, 2:3], stat_t[:, 1:2], (r_stat_,), (r_stat_,))
            k.stt(dst_bf, src_t, stat_t[:, 2:3], wb, ALU.mult, ALU.mult, (r_src, r_stat_, r_wb), (r_dst,))

        def transpose16(src_bf, r_src, dst3, r_dst, col0):
            for half in range(2):
                bt, br = k.bank()
                bv = bt[:].bitcast(BF16)
                for c in range(8):
                    kk = half * 8 + c
                    k.tr(bv[:, c * 128:(c + 1) * 128], src_bf[:, kk * 128:(kk + 1) * 128], ident_b[:], (r_src, r_id), (br,))
                k.cp(dst3[:, half * 8:(half + 1) * 8, col0:col0 + 128], bv.rearrange("p (c t) -> p c t", c=8), (br,),
                     (r_dst,), eng=("act" if half == 0 else "dve"))

        ph45 = ExitStack()
        wq = ph45.enter_context(nc.sbuf_tensor("wq_sb", [128, 16, D], BF16))
        r_wq = s.R("wq")
        with ExitStack() as ph:
            def sb4(name, shape, dt):
                return ph.enter_context(nc.sbuf_tensor(name, list(shape), dt))

            wo = sb4("wo_sb", [128, 16, D], BF16)
            r_wo = s.R("wo")
            wv = wout_d.rearrange("(k p) n -> p k n", p=128)
            for kk in range(0, 16, 4):
                k.dma(wo[:, kk:kk + 4, :], wv[:, kk:kk + 4, :], (), (r_wo,), q="pool")
            wvq = wq_d.rearrange("(k p) n -> p k n", p=128)
            for kk in range(0, 16, 4):
                k.dma(wq[:, kk:kk + 4, :], wvq[:, kk:kk + 4, :], (), (r_wq,), q="pool")
            ffnw = sb4("ffnw_sb", [128, D], F32)
            r_c4 = s.R("consts4")
            k.dma(ffnw[:], ffnw_d.to_broadcast([128, D]), (), (r_c4,))
            cat = [sb4("cat%d" % i, [128, 16, 128], BF16) for i in range(2)]
            r_cat = [s.R("cat", i) for i in range(2)]
            xt4 = [sb4("xt4_%d" % i, [128, D], F32) for i in range(2)]
            r_xt4 = [s.R("xt4", i) for i in range(2)]
            x1t = [sb4("x1t%d" % i, [128, D], F32) for i in range(2)]
            r_x1t = [s.R("x1t", i) for i in range(2)]
            xn2 = [sb4("xn2_%d" % i, [128, D], BF16) for i in range(2)]
            r_xn2 = [s.R("xn2", i) for i in range(2)]
            junk4 = sb4("junk4", [128, D], BF16)
            r_junk4 = s.R("junk4")
            stat4 = [sb4("stat4_%d" % i, [128, 4], F32) for i in range(2)]
            r_stat4 = [s.R("stat4", i) for i in range(2)]
            xT_st = [sb4("xT_st%d" % i, [128, 16, 512], BF16) for i in range(1)]
            r_xT = [s.R("xT_st", i) for i in range(1)]
            def mm4(tt_i):
                sl = tt_i % 2
                k.dma(cat[sl][:], catT_d[:, :, tt_i * 128:(tt_i + 1) * 128], (), (r_cat[sl],))
                k.dma(xt4[sl][:], x_d[tt_i * 128:(tt_i + 1) * 128, :], (), (r_xt4[sl],))
                for nb in range(4):
                    bt, br = k.bank()
                    for kk in range(16):
                        k.mm(bt[:], cat[sl][:, kk, :], wo[:, kk, nb * 512:(nb + 1) * 512], kk == 0, kk == 15,
                             (r_cat[sl], r_wo), (br,))
                    k.tt(x1t[sl][:, nb * 512:(nb + 1) * 512], bt[:], xt4[sl][:, nb * 512:(nb + 1) * 512], ALU.add,
                         (br, r_xt4[sl]), (r_x1t[sl],))
                k.dma(x1_d[tt_i * 128:(tt_i + 1) * 128, :], x1t[sl][:], (r_x1t[sl],), (s.R("x1_d", tt_i),))

            def post4(tt_i):
                sl = tt_i % 2
                st_i, j = tt_i // 4, tt_i % 4
                ssl = 0
                norm_tile(None, x1t[sl][:], r_x1t[sl], stat4[sl], r_stat4[sl], junk4[:], r_junk4, ffnw[:], r_c4,
                          xn2[sl][:], r_xn2[sl])
                transpose16(xn2[sl], r_xn2[sl], xT_st[ssl], r_xT[ssl], j * 128)
                if j == 3:
                    k.dma(xn2T_d[:, :, st_i * 512:(st_i + 1) * 512], xT_st[ssl][:], (r_xT[ssl],), (s.R("xn2T_d", st_i),))

            mm4(0)
            for tt_i in range(NT):
                if tt_i + 1 < NT:
                    mm4(tt_i + 1)
                post4(tt_i)
        s.barrier()

        with ExitStack() as ph:
            def sb5(name, shape, dt):
                return ph.enter_context(nc.sbuf_tensor(name, list(shape), dt))

            skT = sb5("skT_sb", [128, 16, 128], BF16)
            k.dma(skT[:], skT_d, (), (r_wq,), q="pool")
            xst = [sb5("xst%d" % i, [128, 16, 512], BF16) for i in range(2)]
            r_xst = [s.R("xst", i) for i in range(2)]
            qpT = sb5("qpT", [128, 16, 512], BF16)
            r_qpT = s.R("qpT")
            ab = [sb5("ab%d" % i, [128, 16, 128], BF16) for i in range(2)]
            r_ab = [s.R("ab", i) for i in range(2)]
            abm = [sb5("abm%d" % i, [128, 16, 128], BF16) for i in range(2)]
            r_abm = [s.R("abm", i) for i in range(2)]
            m16 = sb5("m16", [128, 16, 16], F32)
            r_m16s = [s.R("m16", i) for i in range(16)]
            tmpk = [sb5("tmpk%d" % i, [128, 128], BF16) for i in range(4)]
            r_tmpks = [s.R("tmpk", i) for i in range(4)]
            cand = [sb5("cand%d" % i, [128, 256], F32) for i in range(4)]
            candk = [sb5("candk%d" % i, [128, 256], F32) for i in range(4)]
            c16 = [sb5("c16_%d" % i, [128, 16], F32) for i in range(4)]
            r_cs = [s.R("cand", i) for i in range(4)]
            thz = [sb5("thz%d" % i, [128, 16], F32) for i in range(2)]
            r_thz = [s.R("thz", i) for i in range(2)]
            zs5 = [sb5("zs5_%d" % i, [128, 4], F32) for i in range(4)]
            thr16 = sb5("thr16", [128, 16], BF16)
            r_thr = s.R("thr16")
            cand4 = [sb5("cand4_%d" % i, [128, 4, 256], F32) for i in range(2)]
            candk4 = [sb5("candk4_%d" % i, [128, 4, 256], F32) for i in range(2)]
            c16g4 = [sb5("c16g4_%d" % i, [128, 4, 16], F32) for i in range(2)]
            zs4 = [sb5("zs4_%d" % i, [128, 8], F32) for i in range(2)]
            a16s4 = [sb5("a16s4_%d" % i, [128, 4, 16], BF16) for i in range(2)]
            r_c4 = [[s.R("c4", i, j) for j in range(4)] for i in range(2)]
            a16s = [sb5("a16s%d" % i, [128, 16], BF16) for i in range(4)]
            r_abms = [[s.R("abm", i, hp) for hp in range(16)] for i in range(2)]
            r_thzs = [[s.R("thz", i, h) for h in range(8)] for i in range(2)]
            for i in range(2):
                k.memset(thz[i][:], 0.0, tuple(r_thzs[i]))
            qpT2 = [qpT, sb5("qpT_b", [128, 16, 512], BF16)]
            r_qpT2 = [r_qpT, s.R("qpT_b")]

            def emit_qp(st_i):
                ssl = st_i % 2
                k.dma(xst[ssl][:], xn2T_d[:, :, st_i * 512:(st_i + 1) * 512], (), (r_xst[ssl],))
                for hp in range(16):
                    bt, br = k.bank()
                    for kk in range(16):
                        k.mm(bt[:], wq[:, kk, hp * 128:(hp + 1) * 128], xst[ssl][:, kk, :], kk == 0, kk == 15,
                             (r_wq, r_xst[ssl]), (br,))
                    k.cp(qpT2[ssl][:, hp, :], bt[:], (br,), (r_qpT2[ssl],), eng="act")

            emit_qp(0)
            for st_i in range(4):
                ssl = st_i % 2
                qpT = qpT2[ssl]
                r_qpT = r_qpT2[ssl]
                for j in range(4):
                    tt_i = st_i * 4 + j
                    sl = tt_i % 2
                    for b4 in range(4):
                        bt, br = k.bank()
                        for i4 in range(4):
                            hp = b4 * 4 + i4
                            k.mm(bt[:, i4 * 128:(i4 + 1) * 128], qpT[:, hp, j * 128:(j + 1) * 128], skT[:, hp, :], True, True,
                                 (r_qpT, r_wq), (br,))
                        k.act(ab[sl][:, b4 * 4:(b4 + 1) * 4, :], bt[:].rearrange("p (a c) -> p a c", c=128), AF.Exp,
                              (br,), (r_ab[sl],))
                    if j == 0 and st_i + 1 < 4:
                        emit_qp(st_i + 1)
                    NI = 4
                    for hp0 in range(0, 16, NI):
                        hps = list(range(hp0, hp0 + NI))
                        for hp in hps:
                            k.max8(m16[:, hp, 0:8], ab[sl][:, hp, :], (r_ab[sl],), (r_m16s[hp],))
                        for hp in hps:
                            k.mrep(tmpk[hp % NI][:], m16[:, hp, 0:8], ab[sl][:, hp, :], 0.0, (r_m16s[hp], r_ab[sl]),
                                   (r_tmpks[hp % NI],))
                        for hp in hps:
                            k.max8(m16[:, hp, 8:16], tmpk[hp % NI][:], (r_tmpks[hp % NI],), (r_m16s[hp],))
                    k.cp(thr16[:], m16[:, :, 15], tuple(r_m16s), (r_thr,))
                    k.tt(abm[sl][:], ab[sl][:], thr16[:].unsqueeze(2).to_broadcast([128, 16, 128]), ALU.is_ge,
                         (r_ab[sl], r_thr), tuple(r_abms[sl]))
                    k.tt(abm[sl][:], abm[sl][:], ab[sl][:], ALU.mult, (r_ab[sl],) + tuple(r_abms[sl]), tuple(r_abms[sl]))
                    m16v = m16[:].rearrange("p (h x) k -> p h x k", x=2)
                    abmv = abm[sl][:].rearrange("p (h x) k -> p h x k", x=2)
                    for gi, h0 in enumerate((0, 4)):
                        hs = list(range(h0, h0 + 4))
                        A = m16v[:, h0:h0 + 4, 0, :]
                        Bm = m16v[:, h0:h0 + 4, 1, :]
                        rA = tuple(r_m16s[2 * h] for h in hs)
                        rB = tuple(r_m16s[2 * h + 1] for h in hs)
                        rc = tuple(r_c4[gi])
                        c4 = cand4[gi]
                        ck4 = candk4[gi]
                        c16g = c16g4[gi]
                        zsg = zs4[gi]

                        def top16():
                            for j in range(4):
                                k.max8(c16g[:, j, 0:8], c4[:, j, :], (r_c4[gi][j],), (r_c4[gi][j],))
                            for j in range(4):
                                k.mrep(ck4[:, j, :], c16g[:, j, 0:8], c4[:, j, :], 0.0, (r_c4[gi][j],), (r_c4[gi][j],))
                            for j in range(4):
                                k.max8(c16g[:, j, 8:16], ck4[:, j, :], (r_c4[gi][j],), (r_c4[gi][j],))

                        k.tt(c4[:].rearrange("p h (a b) -> p h a b", b=16),
                             A.unsqueeze(3).to_broadcast([128, 4, 16, 16]),
                             Bm.unsqueeze(2).to_broadcast([128, 4, 16, 16]), ALU.mult, rA + rB + rc, rc)
                        top16()
                        k.red(zsg[:, 0:4], c16g[:], ALU.add, rc, rc)
                        k.recip(zsg[:, 4:8], zsg[:, 0:4], rc, rc)
                        rabm = tuple(r_abms[sl][2 * h] for h in hs)
                        k.tt(abmv[:, h0:h0 + 4, 0, :], abmv[:, h0:h0 + 4, 0, :],
                             zsg[:, 4:8].unsqueeze(2).to_broadcast([128, 4, 128]), ALU.mult, rc + rabm, rabm)
                        k.tt(a16s4[gi][:], A, zsg[:, 4:8].unsqueeze(2).to_broadcast([128, 4, 16]), ALU.mult,
                             rA + rc, rc)
                        k.tt(c4[:].rearrange("p h (a b) -> p h a b", b=16),
                             a16s4[gi][:].unsqueeze(3).to_broadcast([128, 4, 16, 16]),
                             Bm.unsqueeze(2).to_broadcast([128, 4, 16, 16]), ALU.mult, rB + rc, rc)
                        top16()
                        rth = tuple(r_thzs[sl][h] for h in hs)
                        k.ts(thz[sl][:, h0:h0 + 4], c16g[:, :, 15], 0.99999, None, ALU.mult, None, rc, rth)
                        k.recip(thz[sl][:, 8 + h0:12 + h0], thz[sl][:, h0:h0 + 4], rth, rth)
                    k.dma(ab_d[tt_i * 128:(tt_i + 1) * 128, :], abm[sl][:].rearrange("p a c -> p (a c)"),
                          tuple(r_abms[sl]), (s.R("ab_d", tt_i),))
                    k.dma(thz_d[tt_i * 128:(tt_i + 1) * 128, :], thz[sl][:], tuple(r_thzs[sl]), (s.R("thz_d", tt_i),))
        s.barrier()
        ph45.close()

        with ExitStack() as ph:
            def sb6(name, shape, dt):
                return ph.enter_context(nc.sbuf_tensor(name, list(shape), dt))

            xh = sb6("xh", [128, 16, 1024], BF16)
            acc = sb6("acc", [128, 8, D], F32)
            abh = sb6("abh", [128, 8, 2048], BF16)
            thh = sb6("thh", [128, 8, 16], F32)
            r_xh, r_abh = s.R("xh"), s.R("abh")
            r_acc = [[s.R("acc", i, nb) for nb in range(4)] for i in range(8)]
            ub = [sb6("ub%d" % i, [128, 16, 256], BF16) for i in range(2)]
            vb = [sb6("vb%d" % i, [128, 2, D], BF16) for i in range(2)]
            r_ub = [s.R("ub", i) for i in range(2)]
            r_vb = [s.R("vb", i) for i in range(2)]
            NW = 2
            NT6 = 3
            tmp6 = [sb6("tmp6_%d" % i, [128, 2048], F32) for i in range(NT6)]
            r_tmp6 = [s.R("tmp6", i) for i in range(NT6)]
            msk = [sb6("msk%d" % i, [128, 2048], BF16) for i in range(NW)]
            r_msk = [s.R("msk", i) for i in range(NW)]
            mvb = [sb6("mvb%d" % i, [128, 1024], BF16) for i in range(NW)]
            r_mvb = [s.R("mvb", i) for i in range(NW)]
            ghT = [sb6("ghT%d" % i, [128, 2, 1024], BF16) for i in range(2)]
            r_ghT = [[s.R("ghT", i, q) for q in range(4)] for i in range(2)]
            WhT = [sb6("WhT%d" % i, [128, 256], BF16) for i in range(2)]
            r_WhT = [s.R("WhT", i) for i in range(2)]
            NEB = 64
            def load_half(half):
                t0 = half * 1024
                k.dma(xh[:], xn2T_d[:, :, t0:t0 + 1024], (), (r_xh,))
                k.dma(abh[:], ab_d[t0:t0 + 1024, :].rearrange("(t p) n -> p t n", p=128), (), (r_abh,))
                k.dma(thh[:], thz_d[t0:t0 + 1024, :].rearrange("(t p) n -> p t n", p=128), (), (r_abh,))

            load_half(0)
            for half in range(2):
                t0 = half * 1024
                iters = [(eb, tt_i) for eb in range(NEB) for tt_i in range(8)]

                def load_u(eb):
                    k.dma(ub[eb % 2][:], UT_d[eb], (), (r_ub[eb % 2],), q="pool")

                def load_v(eb):
                    k.dma(vb[eb % 2][:], V_d[eb * 256:(eb + 1) * 256, :].rearrange("(c p) d -> p c d", p=128), (),
                          (r_vb[eb % 2],), q="pool")

                hT_pending = []

                def emit_hT_mm(eb, q):
                    c, grp = q // 2, q % 2
                    bsl = eb % 2
                    bt, br = k.bank()
                    for kk in range(16):
                        k.mm(bt[:], ub[bsl][:, kk, c * 128:(c + 1) * 128], xh[:, kk, grp * 512:(grp + 1) * 512],
                             kk == 0, kk == 15, (r_xh, r_ub[bsl]), (br,))
                    hT_pending.append((eb, q, bt, br))

                def emit_hT_gelu():
                    while hT_pending:
                        eb, q, bt, br = hT_pending.pop(0)
                        c, grp = q // 2, q % 2
                        bsl = eb % 2
                        k.act(ghT[bsl][:, c, grp * 512:(grp + 1) * 512], bt[:], AF.Gelu, (br,), (r_ghT[bsl][q],))

                def emit_outer(it):
                    eb, tt_i = iters[it]
                    w_ = it % NT6
                    av = abh[:, tt_i, :].rearrange("p (h x k) -> p h x k", x=2, k=128)
                    a_ap = av[:, :, 0, 2 * eb:2 * eb + 2]
                    b_ap = av[:, :, 1, :]
                    t4 = tmp6[w_][:].rearrange("p (h a b) -> p h a b", a=2, b=128)
                    k.tt(t4, a_ap.unsqueeze(3).to_broadcast([128, 8, 2, 128]),
                         b_ap.unsqueeze(2).to_broadcast([128, 8, 2, 128]), ALU.mult, (r_abh,), (r_tmp6[w_],), eng="pool")

                def emit_signs(it):
                    eb, tt_i = iters[it]
                    w_ = it % NW
                    t_ = it % NT6
                    for h in range(8):
                        k.act(msk[w_][:, h * 256:(h + 1) * 256], tmp6[t_][:, h * 256:(h + 1) * 256], AF.Sign,
                              (r_tmp6[t_], r_abh), (r_msk[w_],), scale=thh[:, tt_i, 8 + h:9 + h], bias=-1.0)

                def emit_stt(it):
                    w_ = it % NW
                    t_ = it % NT6
                    k.stt(msk[w_][:], msk[w_][:], 0.0, tmp6[t_][:], ALU.max, ALU.mult, (r_tmp6[t_], r_msk[w_]),
                          (r_msk[w_],))
                    k.tt(mvb[w_][:], msk[w_][:, 0:1024], msk[w_][:, 1024:2048], ALU.add, (r_msk[w_],),
                         (r_mvb[w_],))

                def emit_headsum(it):
                    w_ = it % NW
                    btw, brw = k.bank()
                    for c in range(2):
                        for h in range(4):
                            k.mm(btw[:, c * 128:(c + 1) * 128],
                                 mvb[w_][:, h * 256 + c * 128:h * 256 + (c + 1) * 128], ident_b[:], h == 0, h == 3,
                                 (r_mvb[w_], r_id), (brw,))
                    return (btw, brw)

                def emit_WhT(it, bw):
                    eb, tt_i = iters[it]
                    bsl = eb % 2
                    g_ = it % 2
                    btw, brw = bw
                    grp = tt_i // 4
                    k.tt(WhT[g_][:].rearrange("p (c t) -> p c t", c=2), btw[:, 0:256].rearrange("p (c t) -> p c t", c=2),
                         ghT[bsl][:, :, tt_i * 128:(tt_i + 1) * 128], ALU.mult,
                         (brw, r_ghT[bsl][grp], r_ghT[bsl][2 + grp]), (r_WhT[g_],))

                def emit_V(it):
                    eb, tt_i = iters[it]
                    bsl = eb % 2
                    g_ = it % 2
                    vbanks = []
                    for nb in range(4):
                        bt, br = k.bank()
                        for c in range(2):
                            k.mm(bt[:], WhT[g_][:, c * 128:(c + 1) * 128], vb[bsl][:, c, nb * 512:(nb + 1) * 512],
                                 c == 0, c == 1, (r_WhT[g_], r_vb[bsl]), (br,))
                        vbanks.append((bt, br))
                    return vbanks

                def emit_adds(it, vbanks):
                    eb, tt_i = iters[it]
                    for nb in range(4):
                        bt, br = vbanks[nb]
                        dst = acc[:, tt_i, nb * 512:(nb + 1) * 512]
                        ra = r_acc[tt_i][nb]
                        if eb == 0:
                            k.cp(dst, bt[:], (br,), (ra,))
                        else:
                            k.tt(dst, bt[:], dst, ALU.add, (br, ra), (ra,))

                n_it = len(iters)
                load_u(0)
                load_u(1)
                load_v(0)
                emit_outer(0)
                emit_outer(1)
                emit_outer(2)
                emit_signs(0)
                emit_signs(1)
                emit_stt(0)
                for q in range(4):
                    emit_hT_mm(0, q)
                emit_hT_gelu()
                bw_q = {}
                v_q = {}
                for i in range(n_it + 3):
                    if i < n_it:
                        eb, tt_i = iters[i]
                        if tt_i == 3 and eb + 1 < NEB:
                            load_v(eb + 1)
                        if tt_i == 3 and eb + 2 < NEB:
                            load_u(eb + 2)
                        bw_q[i] = emit_headsum(i)
                    if 0 <= i - 1 < n_it:
                        emit_WhT(i - 1, bw_q.pop(i - 1))
                    if 0 <= i - 3 < n_it:
                        emit_adds(i - 3, v_q.pop(i - 3))
                    if 0 <= i - 2 < n_it:
                        v_q[i - 2] = emit_V(i - 2)
                    if i < n_it:
                        eb, tt_i = iters[i]
                        if tt_i % 2 == 0 and eb + 1 < NEB:
                            emit_hT_mm(eb + 1, tt_i // 2)
                    if i + 3 < n_it:
                        emit_outer(i + 3)
                    if i + 2 < n_it:
                        emit_signs(i + 2)
                    emit_hT_gelu()
                    if i + 1 < n_it:
                        emit_stt(i + 1)
                if half == 0:
                    load_half(1)

                def x1_load(tt_i):
                    gt = half * 8 + tt_i
                    sl = tt_i % NT6
                    k.dma(tmp6[sl][:], x1_d[gt * 128:(gt + 1) * 128, :], (), (r_tmp6[sl],))

                for tt_i in range(min(NT6, 8)):
                    x1_load(tt_i)
                for tt_i in range(8):
                    gt = half * 8 + tt_i
                    sl = tt_i % NT6
                    x1v = tmp6[sl][:]
                    k.tt(x1v, x1v, acc[:, tt_i, :], ALU.add, (r_tmp6[sl],) + tuple(r_acc[tt_i]), (r_tmp6[sl],))
                    k.dma(x2_d[gt * 128:(gt + 1) * 128, :], x1v, (r_tmp6[sl],), (s.R("x2_d", gt),))
                    if tt_i + NT6 < 8:
                        x1_load(tt_i + NT6)
        s.barrier()

        with ExitStack() as ph:
            def sb7(name, shape, dt):
                return ph.enter_context(nc.sbuf_tensor(name, list(shape), dt))

            wg = sb7("wg_sb", [128, 16, D], BF16)
            wp = sb7("wp_sb", [128, 2, D], BF16)
            r_wg = s.R("wg")
            wv = wg_d.rearrange("(k p) n -> p k n", p=128)
            for kk in range(0, 16, 4):
                k.dma(wg[:, kk:kk + 4, :], wv[:, kk:kk + 4, :], (), (r_wg,), q="pool")
            k.dma(wp[:], wp_d.rearrange("(k p) n -> p k n", p=128), (), (r_wg,), q="pool")
            plew = sb7("plew_sb", [128, D], F32)
            r_c7 = s.R("consts7")
            k.dma(plew[:], plew_d.to_broadcast([128, D]), (), (r_c7,))
            x2t = [sb7("x2t%d" % i, [128, D], F32) for i in range(2)]
            r_x2t = [s.R("x2t", i) for i in range(2)]
            pt = [sb7("pt%d" % i, [128, 256], F32) for i in range(2)]
            ptb = [sb7("ptb%d" % i, [128, 256], BF16) for i in range(2)]
            r_pt = [s.R("pt", i) for i in range(2)]
            r_ptb = [s.R("ptb", i) for i in range(2)]
            xn3 = [sb7("xn3_%d" % i, [128, D], BF16) for i in range(2)]
            r_xn3 = [s.R("xn3", i) for i in range(2)]
            junk7 = sb7("junk7", [128, D], BF16)
            r_junk7 = s.R("junk7")
            stat7 = [sb7("stat7_%d" % i, [128, 4], F32) for i in range(2)]
            r_stat7 = [s.R("stat7", i) for i in range(2)]
            xn3T = [sb7("xn3T%d" % i, [128, 16, 128], BF16) for i in range(2)]
            r_xn3T = [s.R("xn3T", i) for i in range(2)]
            ppT = [sb7("ppT%d" % i, [128, 2, 128], BF16) for i in range(2)]
            r_ppT = [s.R("ppT", i) for i in range(2)]
            gsb = [sb7("gsb%d" % i, [128, 512], F32) for i in range(2)]
            r_gsb = [s.R("gsb", i) for i in range(2)]
            ot = [sb7("ot%d" % i, [128, D], F32) for i in range(2)]
            r_ot7 = [s.R("ot7", i) for i in range(2)]
            gi = 0

            def prep7(tt_i):
                sl = tt_i % 2
                k.dma(x2t[sl][:], x2_d[tt_i * 128:(tt_i + 1) * 128, :], (), (r_x2t[sl],))
                k.dma(pt[sl][:], p_d[tt_i * 128:(tt_i + 1) * 128, :], (), (r_pt[sl],))
                norm_tile(None, x2t[sl][:], r_x2t[sl], stat7[sl], r_stat7[sl], junk7[:], r_junk7, plew[:], r_c7,
                          xn3[sl][:], r_xn3[sl])
                transpose16(xn3[sl], r_xn3[sl], xn3T[sl], r_xn3T[sl], 0)
                k.cp(ptb[sl][:], pt[sl][:], (r_pt[sl],), (r_ptb[sl],), eng="pool")
                bt, br = k.bank()
                bv = bt[:].bitcast(BF16)
                for c in range(2):
                    k.tr(bv[:, c * 128:(c + 1) * 128], ptb[sl][:, c * 128:(c + 1) * 128], ident_b[:], (r_ptb[sl], r_id), (br,))
                k.cp(ppT[sl][:], bv[:, 0:256].rearrange("p (c t) -> p c t", c=2), (br,), (r_ppT[sl],), eng="act")

            prep7(0)
            for tt_i in range(NT):
                sl = tt_i % 2
                if tt_i + 1 < NT:
                    prep7(tt_i + 1)
                for nb in range(4):
                    g_ = gi % 2
                    gi += 1
                    btg, brg = k.bank()
                    for kk in range(16):
                        k.mm(btg[:], xn3T[sl][:, kk, :], wg[:, kk, nb * 512:(nb + 1) * 512], kk == 0, kk == 15,
                             (r_xn3T[sl], r_wg), (brg,))
                    btp, brp = k.bank()
                    for c in range(2):
                        k.mm(btp[:], ppT[sl][:, c, :], wp[:, c, nb * 512:(nb + 1) * 512], c == 0, c == 1,
                             (r_ppT[sl], r_wg), (brp,))
                    k.act(gsb[g_][:], btg[:], AF.Sigmoid, (brg,), (r_gsb[g_],))
                    k.tt(gsb[g_][:], gsb[g_][:], btp[:], ALU.mult, (r_gsb[g_], brp), (r_gsb[g_],))
                    k.tt(ot[sl][:, nb * 512:(nb + 1) * 512], gsb[g_][:], x2t[sl][:, nb * 512:(nb + 1) * 512], ALU.add,
                         (r_gsb[g_], r_x2t[sl]), (r_ot7[sl],))
                k.dma(out_d[tt_i * 128:(tt_i + 1) * 128, :], ot[sl][:], (r_ot7[sl],), (s.R("out", tt_i),))
        s.barrier()
        s.emit()
    return nc


def _attn_consts(rel_bias):
    f32 = np.float32
    rb = np.asarray(rel_bias, f32)
    sl = np.arange(128)[:, None]
    tl = np.arange(128)[None, :]
    bias_g = np.zeros((128, 6, 4, 4, 128), f32)
    mask_c = np.zeros((128, 6, 4, 4, 128), f32)
    for i in range(5):
        rel = tl - sl + 128 * i
        bk = t5_bucket_np(rel)
        ok = (rel >= 0) & (rel < 512)
        for g in range(4):
            for r in range(4):
                bias_g[:, i, g, r, :] = rb[bk, 4 * g + r]
                mask_c[:, i, g, r, :] = np.where(ok, 0.0, NEG)
    for g in range(4):
        for r in range(4):
            bias_g[:, 5, g, r, :] = rb[31, 4 * g + r]
    tq = np.arange(128)[:, None]
    jj = np.arange(256)[None, :]
    relc = tq - 16 * (jj - 128) - 31
    bkc = t5_bucket_np(relc)
    biasC = np.zeros((128, 16, 256), f32)
    maskC = np.zeros((128, 16, 256), f32)
    for h in range(16):
        biasC[:, h, :] = rb[bkc, h]
        maskC[:, h, :] = np.where(relc >= 0, 0.0, NEG)
    cA = np.zeros((128, 16, 32), f32)
    cB = np.zeros((128, 16, 32), f32)
    blk = np.arange(32)[None, :]
    for qi in range(16):
        t = 128 * qi + np.arange(128)[:, None]
        cur = t // 64
        forced = (blk == 0) | (blk == cur) | (blk == cur - 1)
        valid = blk <= cur
        cA[:, qi, :] = np.where(valid & ~forced, 1.0, 0.0)
        cB[:, qi, :] = np.where(forced, 1e4, np.where(valid, 0.0, -1e30))
    c_start = np.arange(128)[:, None] * 16
    s_start = np.arange(32)[None, :] * 64
    ovl = np.clip(np.minimum(c_start + 32, s_start + 64) - np.maximum(c_start, s_start), 0, None) / 16.0
    ovl = ovl.astype(f32)
    ovl[127, :] = 0.0
    E2 = np.zeros((128, 2048), f32)
    for b_ in range(32):
        E2[b_, b_ * 64:(b_ + 1) * 64] = 1.0
        E2[64 + b_, b_ * 64:(b_ + 1) * 64] = 1.0
    return {
        "bias_g": bias_g.reshape(128, 6, 2048), "mask_c": mask_c.reshape(128, 6, 2048),
        "biasC_g": biasC.reshape(128, 2, 2048), "maskC_c": maskC.reshape(128, 2, 2048),
        "cA": cA.reshape(128, 512), "cB": cB.reshape(128, 512), "ovl": ovl, "E2": E2,
    }


def host_inputs(inputs):
    perm = _perm_cols()
    f32 = np.float32
    w_in = inputs["w_in"][0][:, perm]
    wF = np.ascontiguousarray(w_in[:, 0:1536].reshape(16, 128, 12, 128).transpose(2, 1, 0, 3))
    wT = np.ascontiguousarray(w_in[:, 1536:])
    pools = np.ascontiguousarray(inputs["pool_scale"][0].reshape(8, 128).T)
    rc = np.zeros((4, 16), f32)
    for g in range(4):
        w = 2 << g
        for t in range(16):
            rc[g, t] = 1.0 / min(t + 1, w)
    common = {
        "w_inF": wF,
        "w_inT": wT,
        "mix_norm_w": np.ascontiguousarray(inputs["mix_norm_w"][0:1]),
        "q_norm_w": np.ascontiguousarray(inputs["q_norm_w"][0:1]),
        "k_norm_w": np.ascontiguousarray(inputs["k_norm_w"][0:1]),
        "pool_w": np.ascontiguousarray(inputs["pool_w"][0]),
        "pool_scale": pools,
        "rc16": rc.reshape(1, 64),
        "ident": np.eye(128, dtype=f32),
    }
    common.update(_attn_consts(inputs["rel_bias"]))
    common["w_out"] = np.ascontiguousarray(inputs["w_out"][0])
    common["ffn_norm_w"] = np.ascontiguousarray(inputs["ffn_norm_w"][0:1])
    common["peer_w_q"] = np.ascontiguousarray(inputs["peer_w_q"][0])
    sk = inputs["peer_sub_keys"][0]
    common["skT"] = np.ascontiguousarray(sk.reshape(16, 128, 128).transpose(2, 0, 1))
    U = inputs["peer_u"][0]
    common["peer_UT"] = np.ascontiguousarray(U.reshape(64, 256, 16, 128).transpose(0, 3, 2, 1))
    common["peer_v"] = np.ascontiguousarray(inputs["peer_v"][0])
    common["ple_norm_w"] = np.ascontiguousarray(inputs["ple_norm_w"][0:1])
    common["ple_w_gate"] = np.ascontiguousarray(inputs["ple_w_gate"][0])
    common["ple_w_proj"] = np.ascontiguousarray(inputs["ple_w_proj"][0])
    common["cmp_w1"] = np.ascontiguousarray(np.stack([inputs["cmp_k_w1"][0], inputs["cmp_v_w1"][0]]))
    common["cmp_w2"] = np.ascontiguousarray(np.stack([inputs["cmp_k_w2"][0], inputs["cmp_v_w2"][0]]))
    b1 = np.stack([inputs["cmp_k_b1"][0], inputs["cmp_v_b1"][0]])
    common["cmp_b1"] = np.ascontiguousarray(b1.reshape(2, 2, 128).transpose(2, 0, 1).reshape(128, 4))
    pos = np.stack([inputs["cmp_k_pos"][0], inputs["cmp_v_pos"][0]])
    posT = pos.transpose(2, 0, 1).reshape(64, 64)
    common["cmp_posT"] = np.ascontiguousarray(np.concatenate([posT, posT], axis=0))
    maps = []
    for b in range(8):
        m = dict(common)
        m["x"] = np.ascontiguousarray(inputs["x"][b])
        m["p"] = np.ascontiguousarray(inputs["p"][0, b])
        maps.append(m)
    return maps


def kernel(**inputs):
    inputs = {k_: np.asarray(v) for k_, v in inputs.items()}
    nc = build()
    maps = host_inputs(inputs)
    res = run_bass_kernel_spmd(nc, maps, core_ids=list(range(8)))
    out = np.stack([np.asarray(r["out"]) for r in res.results], axis=0)
    return out.astype(np.float32)
```
